# Optimizing a Trainium2 kernel written in Bass

```python
import math
import jax, jax.numpy as jnp
from jax import lax
import numpy as np

D_MODEL = 2048
BATCH = 8
SEQ = 2048
DEPTH = 1

HEAD_DIM = 128
N_ATTN_HEADS = 12
N_MLSTM_HEADS = 4
ATTN_WIDTH = N_ATTN_HEADS * HEAD_DIM
MLSTM_WIDTH = N_MLSTM_HEADS * HEAD_DIM
MIX_WIDTH = ATTN_WIDTH + MLSTM_WIDTH
WINDOWS = (128, 512, 2048)
DILATIONS = (1, 4, 16)
PAD_MULT = max(WINDOWS)
ROPE_THETA = 10000.0
MLSTM_CHUNK = 128
MLSTM_CONV = 4
D_FF = 5632
FFN_CONV = 3
EPS = 1e-6
IN_WIDTH = 3 * ATTN_WIDTH + 4 * MLSTM_WIDTH + 2 * N_MLSTM_HEADS
IN_SPLITS = (ATTN_WIDTH, 2 * ATTN_WIDTH, 3 * ATTN_WIDTH,
             3 * ATTN_WIDTH + MLSTM_WIDTH, 3 * ATTN_WIDTH + 2 * MLSTM_WIDTH,
             3 * ATTN_WIDTH + 3 * MLSTM_WIDTH, 3 * ATTN_WIDTH + 4 * MLSTM_WIDTH,
             3 * ATTN_WIDTH + 4 * MLSTM_WIDTH + N_MLSTM_HEADS)

kernel_name = 'hymba_mlstm_dilated_attn_convffn'


def _rmsnorm(x, g):
    xf = x.astype(jnp.float32)
    return xf * lax.rsqrt(jnp.mean(xf * xf, axis=-1, keepdims=True) + EPS) * g.astype(jnp.float32)


def _causal_dwconv(x, w, b):
    k_taps = w.shape[0]
    s = x.shape[1]
    xp = jnp.pad(x, ((0, 0), (k_taps - 1, 0), (0, 0)))
    y = b
    for tap in range(k_taps):
        y = y + w[tap] * xp[:, tap:tap + s]
    return y


def _rope(x, pos):
    half = x.shape[-1] // 2
    inv_freq = ROPE_THETA ** (-jnp.arange(half, dtype=jnp.float32) / half)
    ang = pos[:, None] * inv_freq[None, :]
    cos = jnp.cos(ang)[None, :, None, :]
    sin = jnp.sin(ang)[None, :, None, :]
    x1, x2 = x[..., :half], x[..., half:]
    return jnp.concatenate([x1 * cos - x2 * sin, x1 * sin + x2 * cos], axis=-1)


def _dilated_branch(q, k, v, dil, n_back):
    b, h, sp, dh = q.shape
    blk = n_back
    sub = sp // dil
    nb = sub // blk

    def to_blocks(t):
        return t.reshape(b, h, sub, dil, dh).transpose(0, 1, 3, 2, 4).reshape(b, h, dil, nb, blk, dh)

    def with_prev(t):
        prev = jnp.pad(t, ((0, 0), (0, 0), (0, 0), (1, 0), (0, 0), (0, 0)))[:, :, :, :-1]
        return jnp.concatenate([prev, t], axis=4)

    qb = to_blocks(q)
    kb = with_prev(to_blocks(k))
    vb = with_prev(to_blocks(v))
    s = jnp.einsum('bhrnqd,bhrnkd->bhrnqk', qb, kb)
    qi = jnp.arange(blk)[:, None]
    kj = jnp.arange(2 * blk)[None, :]
    dist = blk + qi - kj
    band = (dist >= 0) & (dist <= n_back)
    valid = (jnp.arange(nb)[:, None, None] > 0) | (kj[None] >= blk)
    mask = band[None] & valid
    s = jnp.where(mask, s, -jnp.inf)
    m = jnp.max(s, axis=-1, keepdims=True)
    p = jnp.exp(s - m)
    l = jnp.sum(p, axis=-1, keepdims=True)
    o = jnp.einsum('bhrnqk,bhrnkd->bhrnqd', p, vb) / l
    lse = (m + jnp.log(l))[..., 0]
    o = o.reshape(b, h, dil, sub, dh).transpose(0, 1, 3, 2, 4).reshape(b, h, sp, dh)
    lse = lse.reshape(b, h, dil, sub).transpose(0, 1, 3, 2).reshape(b, h, sp)
    return o, lse


def _dilated_attention(q, k, v, q_g, k_g):
    bsz, s, h, dh = q.shape
    pos = jnp.arange(s, dtype=jnp.float32)
    q = _rope(_rmsnorm(q, q_g), pos) * (dh ** -0.5)
    k = _rope(_rmsnorm(k, k_g), pos)
    v = v.astype(jnp.float32)
    sp = -(-s // PAD_MULT) * PAD_MULT

    def prep(t):
        return jnp.pad(t.transpose(0, 2, 1, 3), ((0, 0), (0, 0), (0, sp - s), (0, 0)))

    q, k, v = prep(q), prep(k), prep(v)
    outs, lses = [], []
    for window, dil in zip(WINDOWS, DILATIONS):
        o, l = _dilated_branch(q, k, v, dil, window // dil)
        outs.append(o)
        lses.append(l)
    wts = jax.nn.softmax(jnp.stack(lses, axis=0), axis=0)
    o = jnp.einsum('gbhs,gbhsd->bhsd', wts, jnp.stack(outs, axis=0))
    return o[:, :, :s].transpose(0, 2, 1, 3).reshape(bsz, s, h * dh)


def _mlstm(q, k, v, i_pre, f_pre):
    bsz, s, h, dh = q.shape
    L = MLSTM_CHUNK
    nc = s // L
    q = q.astype(jnp.float32)
    k = k.astype(jnp.float32) * (dh ** -0.5)
    v = v.astype(jnp.float32)
    log_f = jax.nn.log_sigmoid(f_pre.astype(jnp.float32))
    i_log = i_pre.astype(jnp.float32)

    def chunks(t):
        return t.reshape(bsz, nc, L, h, dh).transpose(1, 0, 3, 2, 4)

    def gchunks(t):
        return t.reshape(bsz, nc, L, h).transpose(1, 0, 3, 2)

    causal = jnp.tril(jnp.ones((L, L), dtype=bool))

    def step(carry, inp):
        c, n, m = carry
        qc, kc, vc, ic, fc = inp
        b = jnp.cumsum(fc, axis=-1)
        a_inter = b + m[..., None]
        d_log = jnp.where(causal, b[..., :, None] - b[..., None, :] + ic[..., None, :], -jnp.inf)
        m_t = jnp.maximum(a_inter, jnp.max(d_log, axis=-1))
        w_intra = jnp.exp(d_log - m_t[..., None])
        w_inter = jnp.exp(a_inter - m_t)
        scores = jnp.einsum('bhtd,bhsd->bhts', qc, kc) * w_intra
        num = w_inter[..., None] * jnp.einsum('bhtd,bhde->bhte', qc, c) + jnp.einsum('bhts,bhse->bhte', scores, vc)
        den = w_inter * jnp.einsum('bhtd,bhd->bht', qc, n) + jnp.sum(scores, axis=-1)
        h_out = num / jnp.maximum(jnp.abs(den), jnp.exp(-m_t))[..., None]
        b_last = b[..., -1]
        g_log = b_last[..., None] - b + ic
        m_new = jnp.maximum(b_last + m, jnp.max(g_log, axis=-1))
        w_k = jnp.exp(g_log - m_new[..., None])
        decay = jnp.exp(b_last + m - m_new)
        kw = w_k[..., None] * kc
        c_new = decay[..., None, None] * c + jnp.einsum('bhsd,bhse->bhde', kw, vc)
        n_new = decay[..., None] * n + jnp.sum(kw, axis=2)
        return (c_new, n_new, m_new), h_out

    init = (jnp.zeros((bsz, h, dh, dh), jnp.float32),
            jnp.zeros((bsz, h, dh), jnp.float32),
            jnp.zeros((bsz, h), jnp.float32))
    _, hs = lax.scan(step, init, (chunks(q), chunks(k), chunks(v), gchunks(i_log), gchunks(log_f)))
    return hs.transpose(1, 0, 3, 2, 4).reshape(bsz, s, h, dh)


def setup_inputs(seed: int = 0) -> dict:
    key = jax.random.key(seed)
    ks = jax.random.split(key, 17)
    f32 = jnp.float32
    nrm = jax.random.normal
    x = nrm(ks[0], (BATCH, SEQ, D_MODEL), f32)
    norm_mix_g = 1.0 + 0.02 * nrm(ks[1], (DEPTH, D_MODEL), f32)
    w_in = nrm(ks[2], (DEPTH, D_MODEL, IN_WIDTH), f32) * D_MODEL ** -0.5
    q_norm_g = 1.0 + 0.02 * nrm(ks[3], (DEPTH, HEAD_DIM), f32)
    k_norm_g = 1.0 + 0.02 * nrm(ks[4], (DEPTH, HEAD_DIM), f32)
    mlstm_conv_w = nrm(ks[5], (DEPTH, MLSTM_CONV, 2 * MLSTM_WIDTH), f32) * MLSTM_CONV ** -0.5
    mlstm_conv_b = 0.02 * nrm(ks[6], (DEPTH, 2 * MLSTM_WIDTH), f32)
    b_igate = 0.1 * nrm(ks[7], (DEPTH, N_MLSTM_HEADS), f32)
    b_fgate = jnp.linspace(3.0, 6.0, N_MLSTM_HEADS, dtype=f32)[None, :] + 0.1 * nrm(ks[8], (DEPTH, N_MLSTM_HEADS), f32)
    mlstm_norm_g = 1.0 + 0.02 * nrm(ks[9], (DEPTH, MLSTM_WIDTH), f32)
    w_out = nrm(ks[10], (DEPTH, MIX_WIDTH, D_MODEL), f32) * MIX_WIDTH ** -0.5
    norm_ffn_g = 1.0 + 0.02 * nrm(ks[11], (DEPTH, D_MODEL), f32)
    w_up = nrm(ks[12], (DEPTH, D_MODEL, 2 * D_FF), f32) * D_MODEL ** -0.5
    ffn_conv_w = nrm(ks[13], (DEPTH, FFN_CONV, 2 * D_FF), f32) * FFN_CONV ** -0.5
    ffn_conv_b = 0.02 * nrm(ks[14], (DEPTH, 2 * D_FF), f32)
    w_down = nrm(ks[15], (DEPTH, D_FF, D_MODEL), f32) * D_FF ** -0.5
    return {'x': x, 'norm_mix_g': norm_mix_g, 'w_in': w_in, 'q_norm_g': q_norm_g, 'k_norm_g': k_norm_g,
            'mlstm_conv_w': mlstm_conv_w, 'mlstm_conv_b': mlstm_conv_b, 'b_igate': b_igate, 'b_fgate': b_fgate,
            'mlstm_norm_g': mlstm_norm_g, 'w_out': w_out, 'norm_ffn_g': norm_ffn_g, 'w_up': w_up,
            'ffn_conv_w': ffn_conv_w, 'ffn_conv_b': ffn_conv_b, 'w_down': w_down}


def reference(x, norm_mix_g, w_in, q_norm_g, k_norm_g, mlstm_conv_w, mlstm_conv_b, b_igate, b_fgate,
              mlstm_norm_g, w_out, norm_ffn_g, w_up, ffn_conv_w, ffn_conv_b, w_down):
    bsz, s, _ = x.shape

    def heads(t, n):
        return t.reshape(bsz, s, n, HEAD_DIM)

    for layer in range(DEPTH):
        h = _rmsnorm(x, norm_mix_g[layer]).astype(x.dtype)
        proj = h @ w_in[layer]
        aq, ak, av, mq, mk, mv, mo, mi, mf = jnp.split(proj, IN_SPLITS, axis=-1)
        attn = _dilated_attention(heads(aq, N_ATTN_HEADS), heads(ak, N_ATTN_HEADS), heads(av, N_ATTN_HEADS),
                                  q_norm_g[layer], k_norm_g[layer])
        qk = jax.nn.silu(_causal_dwconv(jnp.concatenate([mq, mk], axis=-1), mlstm_conv_w[layer], mlstm_conv_b[layer]))
        mq_c, mk_c = jnp.split(qk, 2, axis=-1)
        hm = _mlstm(heads(mq_c, N_MLSTM_HEADS), heads(mk_c, N_MLSTM_HEADS), heads(mv, N_MLSTM_HEADS),
                    mi + b_igate[layer], mf + b_fgate[layer])
        hm = hm * jax.nn.sigmoid(heads(mo, N_MLSTM_HEADS).astype(jnp.float32))
        hm = _rmsnorm(hm, mlstm_norm_g[layer].reshape(N_MLSTM_HEADS, HEAD_DIM)).reshape(bsz, s, MLSTM_WIDTH)
        mix = jnp.concatenate([attn, hm], axis=-1).astype(x.dtype)
        x = x + mix @ w_out[layer]
        h2 = _rmsnorm(x, norm_ffn_g[layer]).astype(x.dtype)
        u = _causal_dwconv(h2 @ w_up[layer], ffn_conv_w[layer], ffn_conv_b[layer])
        gate, val = jnp.split(u, 2, axis=-1)
        x = x + (jax.nn.silu(gate) * val) @ w_down[layer]
    return x
```

```python
import os
import numpy as np
import concourse.bass as bass
import concourse.mybir as mybir
from concourse.bass_utils import run_bass_kernel_spmd

F32 = mybir.dt.float32
BF16 = mybir.dt.bfloat16
AF = mybir.ActivationFunctionType
ALU = mybir.AluOpType

S = 2048
D = 2048
P = 128
KC = 16
TT = 512
NT = 4
NHA = 12
NHM = 4
DFF = 5632
NFC = 44
INW = 6664
EPS = 1e-6
LNSC = float(np.log(128.0 ** -0.5))

CB_ID, CB_ONE, CB_PSG, CB_MOWN, CB_MPREV, CB_M3 = 0, 128, 256, 384, 896, 1408
CB_MOWN4 = 1408 + 4 * 512
CB_MPREV4 = CB_MOWN4 + 512
NCB = CB_MPREV4 + 512
CF_TRIU = 0
CF_G1 = 128
CF_G2 = CF_G1 + 16
CF_QG = CF_G2 + 16
CF_KG = CF_QG + 1
CF_MCW = CF_KG + 1
CF_MCB = CF_MCW + 32
CF_BG = CF_MCB + 8
CF_MNG = CF_BG + 8
CF_FCW = CF_MNG + 4
CF_FCB = CF_FCW + 264
NCF = CF_FCB + 88


class Eng:
    def __init__(self, nc, eng, name):
        self.e = eng
        self.name = name
        self.sem = nc.alloc_semaphore("s_" + name)
        self.cnt = 0
        self.waited = {}

    def wait(self, tok):
        if tok is None:
            return
        s, v = tok
        k = id(s)
        if self.waited.get(k, 0) >= v:
            return
        if self.name == "pe" and s is self.sem:
            return
        self.e.wait_ge(s, v)
        self.waited[k] = v

    def sig(self, ins):
        ins.then_inc(self.sem, 1)
        self.cnt += 1
        return (self.sem, self.cnt)


class Buf:
    def __init__(self, name, excl=False):
        self.name = name
        self.excl = excl
        self.w = None
        self.rs = {}
        self.dsem = None
        self.dcnt = 0

    def add_read(self, tok):
        self.rs[id(tok[0])] = tok

    def set_write(self, tok):
        self.w = tok
        self.rs = {}


class Ctx:
    def __init__(self):
        nc = bass.Bass("TRN2", target_bir_lowering=False)
        self.nc = nc
        self.pe = Eng(nc, nc.tensor, "pe")
        self.act = Eng(nc, nc.scalar, "act")
        self.dve = Eng(nc, nc.vector, "dve")
        self.pool = Eng(nc, nc.gpsimd, "pool")
        self.sp = Eng(nc, nc.sync, "sp")
        self.dbufs = []
        self.nbuf = 0

    def buf(self, name, excl=False):
        return Buf(name, excl)

    def deps(self, eng, reads, writes):
        for b in reads:
            eng.wait(b.w)
            if b.excl:
                for t in list(b.rs.values()):
                    eng.wait(t)
        for b in writes:
            eng.wait(b.w)
            for t in list(b.rs.values()):
                eng.wait(t)

    def mark(self, tok, reads, writes):
        for b in reads:
            if b.excl:
                b.set_write(tok)
            else:
                b.add_read(tok)
        for b in writes:
            b.set_write(tok)

    def op(self, eng, fn, reads=(), writes=()):
        self.deps(eng, reads, writes)
        ins = fn(eng.e)
        tok = eng.sig(ins)
        self.mark(tok, reads, writes)
        return tok

    def mm(self, fns, reads=(), writes=()):
        self.deps(self.pe, reads, writes)
        ins = None
        for f in fns:
            ins = f(self.pe.e)
        tok = self.pe.sig(ins)
        self.mark(tok, reads, writes)
        return tok

    def dma(self, eng, out, in_, sb, load, extra_reads=(), extra_writes=()):
        if sb.dsem is None:
            sb.dsem = self.nc.alloc_semaphore("d%d_%s" % (len(self.dbufs), sb.name))
            self.dbufs.append(sb)
        reads = list(extra_reads) + ([] if load else [sb])
        writes = list(extra_writes) + ([sb] if load else [])
        self.deps(eng, reads, writes)
        if not isinstance(out, (list, tuple)):
            out, in_ = [out], [in_]
        for o_, i_ in zip(out, in_):
            eng.e.dma_start(out=o_, in_=i_).then_inc(sb.dsem, 16)
            sb.dcnt += 16
        tok = (sb.dsem, sb.dcnt)
        self.mark(tok, reads, writes)
        return tok

    def barrier(self, with_pool=False):
        toks = []
        engs = [self.pe, self.act, self.dve, self.sp] + ([self.pool] if with_pool else [])
        for e in [self.pe, self.act, self.dve, self.pool]:
            if e.cnt > 0 and (with_pool or e is not self.pool):
                toks.append((e.sem, e.cnt))
        for b in self.dbufs:
            if b.dcnt > 0 and (with_pool or not b.name.startswith("wslot")):
                toks.append((b.dsem, b.dcnt))
        for e in engs:
            for t in toks:
                e.wait(t)


def build(stage="FULL"):
    K = Ctx()
    nc = K.nc
    pe, act, dve, pool, sp = K.pe, K.act, K.dve, K.pool, K.sp
    dbg = stage != "FULL"

    xT = nc.dram_tensor("xT", [D, S], F32, kind="ExternalInput").ap()
    w_in = nc.dram_tensor("w_in", [D, INW], F32, kind="ExternalInput").ap()
    w_out = nc.dram_tensor("w_out", [D, D], F32, kind="ExternalInput").ap()
    w_up = nc.dram_tensor("w_up", [D, 2 * DFF], F32, kind="ExternalInput").ap()
    w_down = nc.dram_tensor("w_down", [DFF, D], F32, kind="ExternalInput").ap()
    cbd = nc.dram_tensor("cb", [P, NCB], F32, kind="ExternalInput").ap()
    cfd = nc.dram_tensor("cf", [P, NCF], F32, kind="ExternalInput").ap()
    csd = nc.dram_tensor("cs", [P, 2 * S], F32, kind="ExternalInput").ap()
    outT = nc.dram_tensor("outT", [D, S], F32, kind="ExternalOutput").ap()
    mixd = nc.dram_tensor("mixd", [D, S], BF16, kind="ExternalOutput" if dbg else "Internal").ap()
    x1d = nc.dram_tensor("x1d", [D, S], F32, kind="ExternalOutput" if dbg else "Internal").ap()

    w_in_v = w_in.rearrange("(kc p) n -> p kc n", p=P)
    w_out_v = w_out.rearrange("(kc p) n -> p kc n", p=P)
    w_up_v = w_up.rearrange("(kc p) n -> p kc n", p=P)
    w_down_v = w_down.rearrange("(kc p) n -> p kc n", p=P)
    xT_v = xT.rearrange("(kc p) t -> p kc t", p=P)
    mixd_v = mixd.rearrange("(kc p) t -> p kc t", p=P)

    big1 = nc.alloc_sbuf_tensor("big1", [P, 32768], BF16)
    wslots = [nc.alloc_sbuf_tensor("wslot%d" % i, [P, 8192], BF16) for i in range(2)]
    B_ws = [K.buf("wslot%d" % i) for i in range(2)]
    cb = nc.alloc_sbuf_tensor("cbs", [P, NCB], BF16)
    cf = nc.alloc_sbuf_tensor("cfs", [P, NCF], F32)
    wg = nc.alloc_sbuf_tensor("wg", [P, KC, 8], BF16)
    ARENA_B = 95 * 1024
    arena = nc.alloc_sbuf_tensor("arena", [P, ARENA_B // 2], BF16)
    B_cb, B_cf, B_wg = K.buf("cb"), K.buf("cf"), K.buf("wg")

    def aview(boff, nelem, dt):
        assert boff % 4 == 0
        nb = nelem * (4 if dt == F32 else 2)
        assert boff + nb <= ARENA_B, (boff, nb)
        a = arena[:, boff // 2:(boff + nb) // 2]
        return a.bitcast(F32) if dt == F32 else a

    def b1view(boff, nelem, dt):
        nb = nelem * (4 if dt == F32 else 2)
        assert boff + nb <= 65536
        a = big1[:, boff // 2:(boff + nb) // 2]
        return a.bitcast(F32) if dt == F32 else a

    ps = [nc.alloc_psum_tensor("ps%d" % i, [P, 512], F32) for i in range(8)]
    B_ps = [K.buf("ps%d" % i, excl=True) for i in range(8)]

    ident = cb[:, CB_ID:CB_ID + 128]
    ones = cb[:, CB_ONE:CB_ONE + 128]
    psgn = cb[:, CB_PSG:CB_PSG + 128]
    mown = cb[:, CB_MOWN:CB_MOWN + 512]
    mprev = cb[:, CB_MPREV:CB_MPREV + 512]
    triu = cf[:, CF_TRIU:CF_TRIU + 128]

    K.dma(pool, cb[:], cbd, B_cb, True)
    K.dma(sp, cf[:], cfd, B_cf, True)
    K.dma(pool, wg[:], w_in_v[:, :, 6656:6664], B_wg, True)

    blocks = []
    for h in range(NHA):
        blocks.append([(lambda s, i=i: s[:].rearrange("p (k n) -> p k n", n=512)[:, :, i * 128:(i + 1) * 128],
                        w_in_v[:, :, c0:c0 + 128])
                       for i, c0 in enumerate([h * 128, 1536 + h * 128, 3072 + h * 128])])
    for h in range(NHM):
        blocks.append([(lambda s, i=i: s[:].rearrange("p (k n) -> p k n", n=512)[:, :, i * 128:(i + 1) * 128],
                        w_in_v[:, :, c0:c0 + 128])
                       for i, c0 in enumerate([4608 + h * 128, 5120 + h * 128, 5632 + h * 128, 6144 + h * 128])])
    for hf in range(2):
        for jb in range(4):
            blocks.append([(lambda s: s[:].rearrange("p (k n) -> p k n", n=512), w_out_v[:, :, jb * 512:(jb + 1) * 512])])
        for ub in range(22):
            blocks.append([(lambda s: s[:].rearrange("p (k n) -> p k n", n=512)[:, :, 0:256],
                            w_up_v[:, :, ub * 256:(ub + 1) * 256]),
                           (lambda s: s[:].rearrange("p (k n) -> p k n", n=512)[:, :, 256:512],
                            w_up_v[:, :, DFF + ub * 256:DFF + (ub + 1) * 256])])
        for jb2 in range(8):
            for kh in range(2):
                blocks.append([(lambda s: s[:, 0:22 * 256].rearrange("p (k n) -> p k n", n=256),
                                w_down_v[:, kh * 22:(kh + 1) * 22, jb2 * 256:(jb2 + 1) * 256])])
    wstate = {"issued": 0, "used": 0}

    def w_issue():
        i = wstate["issued"]
        if i >= len(blocks):
            return
        sl = i % 2
        K.dma(pool, [vf(wslots[sl]) for vf, src in blocks[i]], [src for vf, src in blocks[i]], B_ws[sl], True)
        wstate["issued"] = i + 1

    def w_next():
        i = wstate["used"]
        wstate["used"] = i + 1
        assert i < wstate["issued"]
        return wslots[i % 2], B_ws[i % 2]

    w_issue()
    w_issue()

    hT = big1[:].rearrange("p (k t) -> p k t", t=S)
    B_hT = K.buf("hT")

    TA = 256
    NTA = S // TA
    xs = [aview(i * 16384, KC * TA, F32).rearrange("p (k t) -> p k t", t=TA) for i in range(4)]
    B_xs = [K.buf("xs%d" % i) for i in range(4)]
    sqA = [aview(65536 + i * 8192, KC * TA, BF16).rearrange("p (k t) -> p k t", t=TA) for i in range(2)]
    B_sqA = [K.buf("sqA0"), K.buf("sqA1")]
    rsA = [aview(81920 + i * 1024, TA, F32) for i in range(2)]
    B_rsA = [K.buf("rsA0"), K.buf("rsA1")]
    for tt in range(3):
        K.dma(sp, xs[tt % 4], xT_v[:, :, tt * TA:(tt + 1) * TA], B_xs[tt % 4], True)
    for tt in range(NTA):
        x_t, B_x = xs[tt % 4], B_xs[tt % 4]
        sq_t, B_sq = sqA[tt % 2], B_sqA[tt % 2]
        rs_t, B_rs = rsA[tt % 2], B_rsA[tt % 2]
        pb = tt % 2
        if tt + 3 < NTA:
            K.dma(sp, xs[(tt + 3) % 4], xT_v[:, :, (tt + 3) * TA:(tt + 4) * TA], B_xs[(tt + 3) % 4], True)
        K.op(act, lambda e: e.activation(out=sq_t, in_=x_t, func=AF.Square), [B_x], [B_sq])
        K.mm([lambda e, kc=kc: e.matmul(ps[pb][:, 0:TA], ones, sq_t[:, kc, :], start=(kc == 0), stop=(kc == KC - 1))
              for kc in range(KC)], [B_sq, B_cb], [B_ps[pb]])
        K.op(act, lambda e: e.activation(out=rs_t, in_=ps[pb][:, 0:TA], func=AF.Ln, scale=1.0 / D, bias=EPS),
             [B_ps[pb]], [B_rs])
        K.op(act, lambda e: e.activation(out=rs_t, in_=rs_t, func=AF.Exp, scale=-0.5), [B_rs], [B_rs])
        for kc in range(KC):
            K.op(dve, lambda e, kc=kc: e.scalar_tensor_tensor(
                out=hT[:, kc, tt * TA:(tt + 1) * TA], in0=x_t[:, kc, :], scalar=cf[:, CF_G1 + kc:CF_G1 + kc + 1],
                in1=rs_t, op0=ALU.mult, op1=ALU.add if False else ALU.mult), [B_x, B_rs, B_cf], [B_hT])

    sp_final = []
    if stage == "A":
        sp_final.append(K.dma(sp, mixd_v, hT, B_hT, False))
        for t in sp_final:
            sp.wait(t)
        return nc

    K.barrier()

    psb = [ps[i].bitcast(BF16) for i in range(8)]
    cs = aview(0, 2 * S, F32)
    B_cs = K.buf("cs")
    K.dma(sp, cs, csd, B_cs, True)
    cos2 = cs[:, 0:S]
    sin2 = cs[:, S:2 * S]
    qTb = [aview(16384 + i * 4096, S, BF16) for i in range(2)]
    kTb = [aview(24576 + i * 4096, S, BF16) for i in range(2)]
    B_qT = [K.buf("qT0"), K.buf("qT1")]
    B_kT = [K.buf("kT0"), K.buf("kT1")]
    vT = aview(32768, S, BF16)
    B_vT = K.buf("vT")
    Vb = [[aview(36864 + (i * 3 + l) * 4096, S, BF16).rearrange("p (b d) -> p b d", d=128) for l in range(3)]
          for i in range(2)]
    B_V = [K.buf("V0"), K.buf("V1")]
    ptb = [aview(87040 + i * 1024, TT, BF16) for i in range(4)]
    B_pt = [K.buf("pt%d" % i) for i in range(4)]
    scr = []
    for i in range(2):
        o = 64512 + i * 8192
        scr.append(dict(sq=aview(o, TT, BF16), qg=aview(o + 1024, TT, BF16), rs=aview(o + 2048, TT, F32),
                        t1=aview(o + 4096, TT, F32), t2=aview(o + 6144, TT, F32),
                        B_sq=K.buf("sq%d" % i), B_qg=K.buf("qg%d" % i), B_rs=K.buf("rs%d" % i),
                        B_t1=K.buf("t1%d" % i), B_t2=K.buf("t2%d" % i)))
    recb = [aview(80896 + i * 2048, TT, F32) for i in range(2)]
    ostb = [aview(80896 + 4096 + i * 1024, TT, BF16) for i in range(2)]
    B_rec = [K.buf("rec0"), K.buf("rec1")]
    B_ost = [K.buf("ost0"), K.buf("ost1")]
    rot = {"mm": 0, "scr": 0, "S": 0, "pt": 0, "o": 0}

    def proj_group(h, slot, B_slot, part, tt):
        hb = h % 2
        sl = slot[:].rearrange("p (k n) -> p k n", n=512)
        bi = rot["mm"] % 2
        rot["mm"] += 1
        bank, B_bank = ps[bi], B_ps[bi]
        K.mm([lambda e, kc=kc: e.matmul(bank[:], sl[:, kc, part * 128:(part + 1) * 128],
                                        hT[:, kc, tt * TT:(tt + 1) * TT], start=(kc == 0), stop=(kc == KC - 1))
              for kc in range(KC)], [B_slot, B_hT], [B_bank])
        tsl = slice(tt * TT, (tt + 1) * TT)
        if part == 2:
            K.op(act, lambda e: e.activation(out=vT[:, tsl], in_=bank[:], func=AF.Copy), [B_bank], [B_vT])
            return None
        sc = scr[rot["scr"] % 2]
        rot["scr"] += 1
        gcol = CF_QG if part == 0 else CF_KG
        dst, B_dst = (qTb[hb], B_qT[hb]) if part == 0 else (kTb[hb], B_kT[hb])
        K.op(act, lambda e: e.activation(out=sc["sq"], in_=bank[:], func=AF.Square), [B_bank], [sc["B_sq"]])
        K.op(act, lambda e: e.activation(out=sc["qg"], in_=bank[:], func=AF.Copy, scale=cf[:, gcol:gcol + 1]),
             [B_bank, B_cf], [sc["B_qg"]])

        def stage2():
            K.mm([lambda e: e.matmul(ps[2][:], ones, sc["sq"], start=True, stop=True)], [sc["B_sq"], B_cb], [B_ps[2]])
            K.mm([lambda e: e.matmul(ps[3][:], psgn, sc["qg"], start=True, stop=True)], [sc["B_qg"], B_cb], [B_ps[3]])
            if part == 0:
                K.op(act, lambda e: e.activation(out=sc["rs"], in_=ps[2][:], func=AF.Ln, scale=1.0, bias=128.0 * EPS),
                     [B_ps[2]], [sc["B_rs"]])
            else:
                K.op(act, lambda e: e.activation(out=sc["rs"], in_=ps[2][:], func=AF.Ln, scale=1.0 / 128.0, bias=EPS),
                     [B_ps[2]], [sc["B_rs"]])
            K.op(act, lambda e: e.activation(out=sc["rs"], in_=sc["rs"], func=AF.Exp, scale=-0.5),
                 [sc["B_rs"]], [sc["B_rs"]])
            K.op(dve, lambda e: e.tensor_tensor(out=sc["t2"], in0=ps[3][:], in1=sin2[:, tsl], op=ALU.mult),
                 [B_ps[3], B_cs], [sc["B_t2"]])
            K.op(dve, lambda e: e.tensor_tensor(out=sc["t1"], in0=sc["qg"], in1=cos2[:, tsl], op=ALU.mult),
                 [sc["B_qg"], B_cs], [sc["B_t1"]])
            K.op(pool, lambda e: e.tensor_tensor(out=sc["t1"], in0=sc["t1"], in1=sc["t2"], op=ALU.add),
                 [sc["B_t1"], sc["B_t2"]], [sc["B_t1"]])
            K.op(pool, lambda e: e.tensor_tensor(out=dst[:, tsl], in0=sc["t1"], in1=sc["rs"], op=ALU.mult),
                 [sc["B_t1"], sc["B_rs"]], [B_dst])
        return stage2

    def v_layouts(h):
        hb = h % 2
        V1, V2, V3 = Vb[hb]
        srcs = []
        for kb in range(16):
            srcs.append((0, kb, vT[:, kb * 128:(kb + 1) * 128]))
        for r in range(4):
            for nb in range(4):
                srcs.append((1, r * 4 + nb, vT[:, nb * 512 + r:(nb + 1) * 512:4]))
        for r in range(16):
            srcs.append((2, r, vT[:, r:S:16]))
        for g in range(6):
            bi = 2 + (g % 2)
            bank = psb[bi][:].rearrange("p (b d) -> p b d", d=128)
            grp = srcs[g * 8:(g + 1) * 8]
            K.mm([lambda e, j=j, src=src: e.transpose(bank[:, j, :], src, ident)
                  for j, (_, _, src) in enumerate(grp)], [B_vT, B_cb], [B_ps[bi]])
            l = grp[0][0]
            b0 = grp[0][1]
            dstv = Vb[hb][l][:, b0:b0 + 8, :]
            K.op(act, lambda e: e.activation(out=dstv, in_=bank, func=AF.Copy), [B_ps[bi]], [B_V[hb]])
            yield

    def attn_tile(h, T):
        hb = h % 2
        qT_, kT_ = qTb[hb], kTb[hb]
        V1, V2, V3 = Vb[hb]
        num, den = ps[6], ps[7]
        num4 = num[:].rearrange("p (j r) -> p r j", r=4)
        num16 = num[:].rearrange("p (j r) -> p r j", r=16)
        den4 = den[:].rearrange("p (j r) -> p r j", r=4)
        den16 = den[:].rearrange("p (j r) -> p r j", r=16)
        t0 = T * TT
        banks = []
        def v4(ap_):
            return ap_.rearrange("p (j r) -> p r j", r=4)

        def v16(ap_):
            return ap_.rearrange("p (j r) -> p r j", r=16)
        fl = lambda i: (lambda x: x[:, 128 * i:128 * (i + 1)])
        r4 = lambda r: (lambda x: v4(x[:, 0:TT])[:, r, :])
        r16 = lambda r: (lambda x: v16(x[:, 0:TT])[:, r, :])
        own1 = [(fl(i), kT_[:, (4 * T + i) * 128:(4 * T + i + 1) * 128],
                 qT_[:, (4 * T + i) * 128:(4 * T + i + 1) * 128]) for i in range(4)]
        pv_own1 = [(fl(i), V1[:, 4 * T + i, :]) for i in range(4)]
        banks.append(("own1", 0, own1, mown, pv_own1))
        i0 = 1 if T == 0 else 0
        prev1 = [(fl(i), kT_[:, (4 * T + i - 1) * 128:(4 * T + i) * 128],
                  qT_[:, (4 * T + i) * 128:(4 * T + i + 1) * 128]) for i in range(i0, 4)]
        pv_prev1 = [(fl(i), V1[:, 4 * T + i - 1, :]) for i in range(i0, 4)]
        banks.append(("prev1", 128 * i0, prev1, mprev, pv_prev1))
        own2 = [(r4(r), kT_[:, t0 + r:t0 + TT:4], qT_[:, t0 + r:t0 + TT:4]) for r in range(4)]
        pv_own2 = [(r4(r), V2[:, r * 4 + T, :]) for r in range(4)]
        banks.append(("own2", 0, own2, cb[:, CB_MOWN4:CB_MOWN4 + 512], pv_own2))
        if T >= 1:
            prev2 = [(r4(r), kT_[:, t0 - TT + r:t0:4], qT_[:, t0 + r:t0 + TT:4]) for r in range(4)]
            pv_prev2 = [(r4(r), V2[:, r * 4 + T - 1, :]) for r in range(4)]
            banks.append(("prev2", 0, prev2, cb[:, CB_MPREV4:CB_MPREV4 + 512], pv_prev2))
        b3 = [(r16(r), kT_[:, r:S:16], qT_[:, t0 + r:t0 + TT:16]) for r in range(16)]
        pv_b3 = [(r16(r), V3[:, r, :]) for r in range(16)]
        banks.append(("b3", 0, b3, cb[:, CB_M3 + T * 512:CB_M3 + (T + 1) * 512], pv_b3))
        return banks

    def attn_head(h):
        hb = h % 2
        num, den = ps[6], ps[7]
        den4 = den[:].rearrange("p (j r) -> p r j", r=4)
        den16 = den[:].rearrange("p (j r) -> p r j", r=16)
        allb = []
        for T in range(NT):
            bl = attn_tile(h, T)
            for k, bk in enumerate(bl):
                allb.append((T, k == 0, k == len(bl) - 1, bk))
        pend = []

        def do_pv(item):
            T, first, last, (name, c0, smm, mask, pvl), pt, B_p = item
            fns = []
            for k, (colf, lhsT) in enumerate(pvl):
                st = first and k == 0
                fns.append(lambda e, colf=colf, lhsT=lhsT, st=st: e.matmul(
                    colf(num), lhsT, colf(pt), start=st, stop=False, skip_group_check=True))
            fns.append(lambda e, st=first: e.matmul(den[:, c0:TT], ones, pt[:, c0:TT], start=st, stop=False,
                                                    skip_group_check=True))
            K.mm(fns, [B_p, B_V[hb], B_cb], [B_ps[6], B_ps[7]])
            if last:
                t0 = T * TT
                oi = rot["o"] % 2
                rot["o"] += 1
                K.op(dve, lambda e: e.reciprocal(out=recb[oi], in_=den[:]), [B_ps[7]], [B_rec[oi]])
                K.op(dve, lambda e: e.tensor_tensor(out=ostb[oi], in0=num[:], in1=recb[oi], op=ALU.mult),
                     [B_ps[6], B_rec[oi]], [B_ost[oi]])
                K.dma(sp, mixd[h * 128:(h + 1) * 128, t0:t0 + TT], ostb[oi], B_ost[oi], False)

        for (T, first, last, bk) in allb:
            (name, c0, smm, mask, pvl) = bk
            si = 4 + rot["S"] % 2
            rot["S"] += 1
            pi = rot["pt"] % 4
            rot["pt"] += 1
            Sb, B_S = ps[si], B_ps[si]
            pt, B_p = ptb[pi], B_pt[pi]
            K.mm([lambda e, a=a: e.matmul(a[0](Sb), a[1], a[2], start=True, stop=True)
                  for a in smm], [B_qT[hb], B_kT[hb]], [B_S])
            K.op(act, lambda e: e.activation(out=pt[:, c0:TT], in_=Sb[:, c0:TT], func=AF.Exp), [B_S], [B_p])
            K.op(pool, lambda e: e.tensor_tensor(out=pt[:, c0:TT], in0=pt[:, c0:TT], in1=mask[:, c0:TT], op=ALU.mult),
                 [B_p, B_cb], [B_p])
            pend.append((T, first, last, bk, pt, B_p))
            if len(pend) > 2:
                do_pv(pend.pop(0))
            yield
        while pend:
            do_pv(pend.pop(0))
            yield

    nheads = NHA if stage != "B1" else 2
    for h in range(nheads + 1):
        yth = attn_head(h - 1) if h >= 1 else None

        def ystep(n):
            nonlocal yth
            for _ in range(n):
                if yth is None:
                    return
                try:
                    next(yth)
                except StopIteration:
                    yth = None
        if h < nheads:
            slot, B_slot = w_next()
            prev2 = None
            for g in range(12):
                st2 = proj_group(h, slot, B_slot, g // 4, g % 4)
                if prev2 is not None:
                    prev2()
                prev2 = st2
                ystep(2)
            w_issue()
            for _ in v_layouts(h):
                ystep(1)
        ystep(1000)

    if stage in ("B", "B1"):
        for b in K.dbufs:
            if b.dcnt > 0:
                sp.wait((b.dsem, b.dcnt))
        return nc
    K.barrier()

    def bc_ap(base, dims):
        return bass.AP(base.tensor, base.offset, [list(base.ap[0])] + [list(d) for d in dims])

    XPW = 2056
    xp = [aview(i * XPW * 4, XPW, F32) for i in range(2)]
    B_xp = [K.buf("xpq"), K.buf("xpk")]
    accv = aview(2 * XPW * 4, S, F32)
    B_acc = K.buf("acc")
    o0 = 24640
    mqT = aview(o0, 4 * S, BF16).rearrange("p (h t) -> p h t", t=S)
    mkT = aview(o0 + 16384, 4 * S, BF16).rearrange("p (h t) -> p h t", t=S)
    mvt = aview(o0 + 32768, 16 * 512, BF16).rearrange("p (c n) -> p c n", n=512)
    sgo = aview(o0 + 49152, 4 * S, BF16).rearrange("p (h t) -> p h t", t=S)
    B_mq, B_mk, B_mv, B_sgo = K.buf("mqT"), K.buf("mkT"), K.buf("mvt"), K.buf("sgo")
    o1 = o0 + 65536
    gt = aview(o1, 128, F32).rearrange("p (c g) -> p c g", g=8)
    e1 = aview(o1 + 512, 64, F32).rearrange("p (c g) -> p c g", g=4)
    lf = aview(o1 + 768, 64, F32).rearrange("p (c g) -> p c g", g=4)
    ib = aview(o1 + 1024, 64, F32).rearrange("p (c g) -> p c g", g=4)
    ebias = aview(o1 + 1280, 64, F32).rearrange("p (c g) -> p c g", g=4)
    B_gt, B_e1, B_lf, B_ib, B_eb = [K.buf(n) for n in ("gt", "e1", "lf", "ib", "ebias")]

    for i in range(2):
        K.op(dve, lambda e, i=i: e.memset(xp[i][:, 0:3], 0.0), [], [B_xp[i]])

    for c in range(16):
        K.mm([lambda e, kc=kc, c=c: e.matmul(ps[7][:, c * 8:(c + 1) * 8], hT[:, kc, c * 128:(c + 1) * 128], wg[:, kc, :],
                                             start=(kc == 0), stop=(kc == KC - 1)) for kc in range(KC)],
             [B_hT, B_wg], [B_ps[7]])
    bgb = bc_ap(cf[:, CF_BG:CF_BG + 8], [[0, 16], [1, 8]])
    K.op(dve, lambda e: e.tensor_tensor(out=gt, in0=ps[7][:, 0:128].rearrange("p (c g) -> p c g", g=8), in1=bgb,
                                        op=ALU.add), [B_ps[7], B_cf], [B_gt])
    K.op(act, lambda e: e.activation(out=e1, in_=gt[:, :, 4:8], func=AF.Exp, scale=-1.0), [B_gt], [B_e1])
    K.op(act, lambda e: e.activation(out=e1, in_=e1, func=AF.Ln, bias=1.0, scale=1.0), [B_e1], [B_e1])
    K.op(dve, lambda e: e.tensor_scalar(out=lf, in0=e1, scalar1=-1.0, scalar2=None, op0=ALU.mult), [B_e1], [B_lf])
    K.mm([lambda e, c=c: e.matmul(ps[6][:, c * 4:(c + 1) * 4], triu, lf[:, c, :], start=True, stop=True)
          for c in range(16)], [B_lf, B_cf], [B_ps[6]])
    K.op(dve, lambda e: e.scalar_tensor_tensor(out=ib, in0=gt[:, :, 0:4], scalar=LNSC,
                                               in1=ps[6][:, 0:64].rearrange("p (c g) -> p c g", g=4),
                                               op0=ALU.add, op1=ALU.subtract), [B_gt, B_ps[6]], [B_ib])
    K.op(act, lambda e: e.activation(out=ebias, in_=ib, func=AF.Exp), [B_ib], [B_eb])

    for hm in range(NHM):
        slot, B_slot = w_next()
        sl = slot[:].rearrange("p (k n) -> p k n", n=512)
        for part in range(2):
            for tt in range(NT):
                bi = rot["mm"] % 2
                rot["mm"] += 1
                K.mm([lambda e, kc=kc: e.matmul(ps[bi][:], sl[:, kc, part * 128:(part + 1) * 128],
                                                hT[:, kc, tt * TT:(tt + 1) * TT], start=(kc == 0), stop=(kc == KC - 1))
                      for kc in range(KC)], [B_slot, B_hT], [B_ps[bi]])
                K.op(act, lambda e: e.activation(out=xp[part][:, 3 + tt * TT:3 + (tt + 1) * TT], in_=ps[bi][:],
                                                 func=AF.Copy), [B_ps[bi]], [B_xp[part]])
            j = part * 4 + hm
            wc = lambda k: cf[:, CF_MCW + j * 4 + k:CF_MCW + j * 4 + k + 1]
            K.op(dve, lambda e: e.tensor_scalar(out=accv, in0=xp[part][:, 3:3 + S], scalar1=wc(3),
                                                scalar2=cf[:, CF_MCB + j:CF_MCB + j + 1], op0=ALU.mult, op1=ALU.add),
                 [B_xp[part], B_cf], [B_acc])
            for k in (2, 1, 0):
                K.op(dve, lambda e, k=k: e.scalar_tensor_tensor(out=accv, in0=xp[part][:, k:k + S], scalar=wc(k),
                                                                in1=accv, op0=ALU.mult, op1=ALU.add),
                     [B_xp[part], B_cf, B_acc], [B_acc])
            dstT, B_d = (mqT, B_mq) if part == 0 else (mkT, B_mk)
            K.op(act, lambda e: e.activation(out=dstT[:, hm, :], in_=accv, func=AF.Silu), [B_acc], [B_d])
        for g in range(4):
            bi = rot["mm"] % 2
            rot["mm"] += 1
            fns = []
            for jj in range(4):
                c = g * 4 + jj
                for kc in range(KC):
                    fns.append(lambda e, kc=kc, c=c, jj=jj: e.matmul(
                        ps[bi][:, jj * 128:(jj + 1) * 128], hT[:, kc, c * 128:(c + 1) * 128], sl[:, kc, 256:384],
                        start=(kc == 0), stop=(kc == KC - 1)))
            K.mm(fns, [B_slot, B_hT], [B_ps[bi]])
            K.op(act, lambda e: e.activation(out=mvt[:, g * 4:(g + 1) * 4, hm * 128:(hm + 1) * 128],
                                             in_=ps[bi][:].rearrange("p (j d) -> p j d", d=128), func=AF.Copy),
                 [B_ps[bi]], [B_mv])
        for tt in range(NT):
            bi = rot["mm"] % 2
            rot["mm"] += 1
            K.mm([lambda e, kc=kc: e.matmul(ps[bi][:], sl[:, kc, 384:512], hT[:, kc, tt * TT:(tt + 1) * TT],
                                            start=(kc == 0), stop=(kc == KC - 1)) for kc in range(KC)],
                 [B_slot, B_hT], [B_ps[bi]])
            K.op(act, lambda e: e.activation(out=sgo[:, hm, tt * TT:(tt + 1) * TT], in_=ps[bi][:], func=AF.Sigmoid),
                 [B_ps[bi]], [B_sgo])
        w_issue()

    K.barrier()
    b1o = [0]

    def b1a(nelem, dt):
        nb = nelem * (4 if dt == F32 else 2)
        o = b1o[0]
        b1o[0] = o + ((nb + 31) // 32) * 32
        return b1view(o, nelem, dt)

    def v4(dt=F32):
        return b1a(512, dt).rearrange("p (h s) -> p h s", s=128)
    lfrep = v4()
    Et3 = [v4() for _ in range(3)]
    dm = v4()
    st2 = [b1a(512, BF16) for _ in range(2)]
    qs2 = [v4(BF16) for _ in range(2)]
    dn2 = [b1a(512, F32) for _ in range(4)]
    nS2 = [b1a(512, F32) for _ in range(4)]
    hg2 = [v4() for _ in range(2)]
    sqh = b1a(512, BF16)
    rsm = v4()
    hmst = v4(BF16)
    wk2 = [b1a(4, F32) for _ in range(2)]
    kw2 = [v4(BF16) for _ in range(2)]
    Cst = b1a(1024, F32).rearrange("p (h n) -> p h n", n=256)
    Cbf = b1a(1024, BF16).rearrange("p (h n) -> p h n", n=256)
    (B_lfrep, B_dm, B_sqh, B_rsm, B_hmst, B_Cst, B_Cbf) = [
        K.buf(n) for n in ("lfrep", "dm", "sqh", "rsm", "hmst", "Cst", "Cbf")]
    B_E3 = [K.buf("E%d" % i) for i in range(3)]
    B_st2 = [K.buf("st0"), K.buf("st1")]
    B_qs2 = [K.buf("qs0"), K.buf("qs1")]
    B_dn2 = [K.buf("dn%d" % i) for i in range(4)]
    B_nS2 = [K.buf("nS%d" % i) for i in range(4)]
    B_hg2 = [K.buf("hg0"), K.buf("hg1")]
    B_wk2 = [K.buf("wk0"), K.buf("wk1")]
    B_kw2 = [K.buf("kw0"), K.buf("kw1")]
    mown128 = cb[:, CB_MOWN:CB_MOWN + 128]
    mixm = mixd[1536:2048, :].rearrange("(h p) t -> p h t", p=P)
    ktp = psb[5][:, 0:512].rearrange("p (h d) -> p h d", d=128)
    NCH = 16

    def st_P1(c):
        csl = slice(c * 128, (c + 1) * 128)
        Et, B_E = Et3[c % 3], B_E3[c % 3]
        K.op(dve, lambda e: e.tensor_copy(out=lfrep, in_=bc_ap(lf[:, c, :], [[1, 4], [0, 128]])), [B_lf], [B_lfrep])
        K.mm([lambda e, h=h: e.matmul(ps[0][:, h * 128:(h + 1) * 128], lfrep[:, h, :], triu, start=True, stop=True)
              for h in range(4)], [B_lfrep, B_cf], [B_ps[0]])
        K.op(act, lambda e: e.activation(out=Et, in_=ps[0][:].rearrange("p (h s) -> p h s", s=128), func=AF.Exp),
             [B_ps[0]], [B_E])

    def st_P1b(c):
        csl = slice(c * 128, (c + 1) * 128)
        K.mm([lambda e, h=h: e.matmul(ps[1][:, h * 128:(h + 1) * 128], mkT[:, h, csl], mqT[:, h, csl],
                                      start=True, stop=True) for h in range(4)], [B_mk, B_mq], [B_ps[1]])
        if c < NCH - 1:
            K.mm([lambda e, h=h: e.transpose(ktp[:, h, :], mkT[:, h, csl], ident) for h in range(4)],
                 [B_mk, B_cb], [B_ps[5]])

    def st_P2(c):
        cb_ = c % 2
        csl = slice(c * 128, (c + 1) * 128)
        Et, B_E = Et3[c % 3], B_E3[c % 3]
        K.op(dve, lambda e: e.tensor_tensor(out=dm[:].rearrange("p h s -> p (h s)"), in0=Et[:].rearrange("p h s -> p (h s)"),
                                            in1=mown, op=ALU.mult), [B_E, B_cb], [B_dm])
        K.op(dve, lambda e: e.tensor_tensor(out=dm, in0=dm, in1=bc_ap(ebias[:, c, :], [[1, 4], [0, 128]]), op=ALU.mult),
             [B_dm, B_eb], [B_dm])
        K.op(dve, lambda e: e.tensor_tensor(out=st2[cb_], in0=ps[1][:], in1=dm[:].rearrange("p h s -> p (h s)"),
                                            op=ALU.mult), [B_ps[1], B_dm], [B_st2[cb_]])
        K.op(dve, lambda e: e.tensor_tensor(out=qs2[cb_], in0=mqT[:, :, csl], in1=Et, op=ALU.mult),
             [B_mq, B_E], [B_qs2[cb_]])
        if c < NCH - 1:
            K.op(dve, lambda e: e.tensor_tensor(out=wk2[cb_], in0=ebias[:, c, :], in1=Et[:, :, 127], op=ALU.mult),
                 [B_eb, B_E], [B_wk2[cb_]])
            for h in range(4):
                K.op(act, lambda e, h=h: e.activation(out=kw2[cb_][:, h, :], in_=ktp[:, h, :], func=AF.Copy,
                                                      scale=wk2[cb_][:, h:h + 1]), [B_ps[5], B_wk2[cb_]], [B_kw2[cb_]])

    def st_R(c):
        cb_ = c % 2
        Et, B_E = Et3[c % 3], B_E3[c % 3]
        st_, qs_, kw_ = st2[cb_], qs2[cb_], kw2[cb_]
        if c < NCH - 1:
            fns = []
            for h in range(4):
                bk = ps[6 + h // 2]
                o = (h % 2) * 256
                fns.append(lambda e, h=h, bk=bk, o=o: e.matmul(bk[:, o:o + 128], kw_[:, h, :], mvt[:, c, h * 128:(h + 1) * 128],
                                                               start=True, stop=True))
                fns.append(lambda e, h=h, bk=bk, o=o: e.matmul(bk[:, o + 128:o + 256], kw_[:, h, :], ones, start=True, stop=True))
            K.mm(fns, [B_kw2[cb_], B_mv, B_cb], [B_ps[6], B_ps[7]])
        fns = []
        for h in range(4):
            hs = slice(h * 128, (h + 1) * 128)
            if c > 0:
                fns.append(lambda e, h=h, hs=hs: e.matmul(ps[2][:, hs], Cbf[:, h, 0:128], qs_[:, h, :], start=True, stop=False))
            fns.append(lambda e, h=h, hs=hs: e.matmul(ps[2][:, hs], mvt[:, c, hs], st_[:, hs], start=(c == 0), stop=True))
            if c > 0:
                fns.append(lambda e, h=h, hs=hs: e.matmul(ps[3][:, hs], Cbf[:, h, 128:256], qs_[:, h, :], start=True, stop=False))
            fns.append(lambda e, h=h, hs=hs: e.matmul(ps[3][:, hs], ones, st_[:, hs], start=(c == 0), stop=True))
        K.mm(fns, [B_Cbf, B_qs2[cb_], B_mv, B_st2[cb_], B_cb], [B_ps[2], B_ps[3]])
        if c < NCH - 1:
            for h in range(4):
                bk = ps[6 + h // 2]
                o = (h % 2) * 256
                if c == 0:
                    K.op(dve, lambda e, h=h, bk=bk, o=o: e.tensor_copy(out=Cst[:, h, :], in_=bk[:, o:o + 256]),
                         [B_ps[6 + h // 2]], [B_Cst])
                else:
                    K.op(dve, lambda e, h=h, bk=bk, o=o: e.scalar_tensor_tensor(
                        out=Cst[:, h, :], in0=Cst[:, h, :], scalar=Et[:, h, 127:128], in1=bk[:, o:o + 256],
                        op0=ALU.mult, op1=ALU.add), [B_Cst, B_E, B_ps[6 + h // 2]], [B_Cst])
            K.op(act, lambda e: e.activation(out=Cbf, in_=Cst, func=AF.Copy), [B_Cst], [B_Cbf])
        K.op(act, lambda e: e.activation(out=dn2[c % 4], in_=ps[3][:], func=AF.Abs), [B_ps[3]], [B_dn2[c % 4]])
        K.op(act, lambda e: e.activation(out=nS2[c % 4], in_=ps[2][:], func=AF.Copy), [B_ps[2]], [B_nS2[c % 4]])

    def st_T1a(c):
        cb_ = c % 4
        dn = dn2[cb_]
        K.op(dve, lambda e: e.tensor_scalar(out=dn, in0=dn, scalar1=1.0, scalar2=None, op0=ALU.max), [B_dn2[cb_]], [B_dn2[cb_]])
        K.op(act, lambda e: e.activation(out=dn, in_=dn, func=AF.Ln), [B_dn2[cb_]], [B_dn2[cb_]])
        K.op(act, lambda e: e.activation(out=dn, in_=dn, func=AF.Exp, scale=-1.0), [B_dn2[cb_]], [B_dn2[cb_]])

    def st_T1b(c):
        cb_ = c % 4
        csl = slice(c * 128, (c + 1) * 128)
        dn, hg = dn2[cb_], hg2[c % 2]
        K.op(dve, lambda e: e.tensor_tensor(out=hg[:].rearrange("p h s -> p (h s)"), in0=nS2[cb_], in1=dn, op=ALU.mult),
             [B_nS2[cb_], B_dn2[cb_]], [B_hg2[c % 2]])
        K.op(dve, lambda e: e.tensor_tensor(out=hg, in0=hg, in1=sgo[:, :, csl], op=ALU.mult), [B_hg2[c % 2], B_sgo], [B_hg2[c % 2]])
        K.op(act, lambda e: e.activation(out=sqh, in_=hg[:].rearrange("p h s -> p (h s)"), func=AF.Square),
             [B_hg2[c % 2]], [B_sqh])

    def st_T2a(c):
        K.mm([lambda e: e.matmul(ps[4][:], ones, sqh, start=True, stop=True)], [B_sqh, B_cb], [B_ps[4]])
        K.op(act, lambda e: e.activation(out=rsm[:].rearrange("p h s -> p (h s)"), in_=ps[4][:], func=AF.Ln,
                                         scale=1.0 / 128.0, bias=EPS), [B_ps[4]], [B_rsm])
        K.op(act, lambda e: e.activation(out=rsm, in_=rsm, func=AF.Exp, scale=-0.5), [B_rsm], [B_rsm])

    def st_T2b(c):
        cb_ = c % 2
        csl = slice(c * 128, (c + 1) * 128)
        hg = hg2[cb_]
        K.op(dve, lambda e: e.tensor_tensor(out=hg, in0=hg, in1=rsm, op=ALU.mult), [B_hg2[cb_], B_rsm], [B_hg2[cb_]])
        K.op(dve, lambda e: e.tensor_tensor(out=hmst, in0=hg, in1=bc_ap(cf[:, CF_MNG:CF_MNG + 4], [[1, 4], [0, 128]]),
                                            op=ALU.mult), [B_hg2[cb_], B_cf], [B_hmst])
        K.dma(sp, mixm[:, :, csl], hmst, B_hmst, False)

    def ok(c):
        return 0 <= c < NCH
    for it in range(-2, NCH + 4):
        if ok(it):
            st_R(it)
        if ok(it - 3):
            st_T2a(it - 3)
        if ok(it + 2):
            st_P1(it + 2)
        if ok(it + 1):
            st_P2(it + 1)
        if ok(it - 3):
            st_T2b(it - 3)
        if ok(it + 2):
            st_P1b(it + 2)
        if ok(it - 1):
            st_T1a(it - 1)
        if ok(it - 2):
            st_T1b(it - 2)

    if stage == "B2":
        for b in K.dbufs:
            if b.dcnt > 0:
                sp.wait((b.dsem, b.dcnt))
        return nc

    HS = 1024
    mixh = b1view(0, KC * HS, BF16).rearrange("p (k t) -> p k t", t=HS)
    h2p = b1view(32768, KC * HS, BF16).rearrange("p (k t) -> p k t", t=HS)
    B_mixh, B_h2p = K.buf("mixh"), K.buf("h2p")
    actT = aview(0, NFC * HS, BF16).rearrange("p (c t) -> p c t", t=HS)
    B_actT = K.buf("actT")
    rs2 = aview(90112, HS, F32)
    halo = aview(94208, 176, F32).rearrange("p (j w) -> p j w", w=2)
    B_rs2, B_halo = K.buf("rs2"), K.buf("halo")
    xr = [aview(i * 2048, TT, F32) for i in range(4)]
    x1t = [aview(8192 + i * 2048, TT, F32) for i in range(2)]
    sq2 = [aview(12288 + i * 1024, TT, BF16) for i in range(2)]
    B_xr = [K.buf("xr%d" % i) for i in range(4)]
    B_x1t = [K.buf("x1t%d" % i) for i in range(2)]
    B_sq2 = [K.buf("sq2%d" % i) for i in range(2)]
    YW = 1026
    ybuf = [[b1view((si * 2 + gv) * YW * 4, YW, F32) for gv in range(2)] for si in range(2)]
    B_y = [[K.buf("y%d%d" % (si, gv)) for gv in range(2)] for si in range(2)]
    ag = b1view(4 * YW * 4, HS, F32)
    av = b1view(4 * YW * 4 + 4096, HS, F32)
    B_ag, B_av = K.buf("ag"), K.buf("av")
    xr2 = [b1view(4 * YW * 4 + 8192 + i * 2048, TT, F32) for i in range(2)]
    B_xr2 = [K.buf("xr20"), K.buf("xr21")]
    otb = [ag[:, 0:TT], av[:, 0:TT]]
    B_ot = [B_ag, B_av]

    for hf in range(2):
        hoff = hf * HS
        K.barrier()
        K.dma(sp, mixh, mixd_v[:, :, hoff:hoff + HS], B_mixh, True)
        steps = [(jb, jc4, t2) for jb in range(4) for jc4 in range(4) for t2 in range(2)]

        def xload(i):
            jb, jc4, t2 = steps[i]
            jc = jb * 4 + jc4
            K.dma(sp, xr[i % 4], xT[jc * 128:(jc + 1) * 128, hoff + t2 * TT:hoff + (t2 + 1) * TT], B_xr[i % 4], True)
        xload(0)
        xload(1)
        slot = None
        pend_ssq = None
        for i, (jb, jc4, t2) in enumerate(steps):
            jc = jb * 4 + jc4
            if jc4 == 0 and t2 == 0:
                slot, B_slot = w_next()
                sl = slot[:].rearrange("p (k n) -> p k n", n=512)
            if i + 2 < len(steps):
                xload(i + 2)
            bi = rot["mm"] % 4
            rot["mm"] += 1
            K.mm([lambda e, kc=kc: e.matmul(ps[bi][:], sl[:, kc, jc4 * 128:(jc4 + 1) * 128],
                                            mixh[:, kc, t2 * TT:(t2 + 1) * TT], start=(kc == 0), stop=(kc == KC - 1))
                  for kc in range(KC)], [B_slot, B_mixh], [B_ps[bi]])
            xi = i % 2
            K.op(dve, lambda e: e.tensor_tensor(out=x1t[xi], in0=ps[bi][:], in1=xr[i % 4], op=ALU.add),
                 [B_ps[bi], B_xr[i % 4]], [B_x1t[xi]])
            K.dma(sp, x1d[jc * 128:(jc + 1) * 128, hoff + t2 * TT:hoff + (t2 + 1) * TT], x1t[xi], B_x1t[xi], False)
            K.op(act, lambda e: e.activation(out=h2p[:, jc, t2 * TT:(t2 + 1) * TT], in_=x1t[xi], func=AF.Copy,
                                             scale=cf[:, CF_G2 + jc:CF_G2 + jc + 1]), [B_x1t[xi], B_cf], [B_h2p])
            K.op(act, lambda e: e.activation(out=sq2[xi], in_=x1t[xi], func=AF.Square), [B_x1t[xi]], [B_sq2[xi]])
            if pend_ssq is not None:
                pend_ssq()
            pend_ssq = (lambda t2=t2, xi=xi, jc=jc: K.mm(
                [lambda e: e.matmul(ps[6 + t2][:], ones, sq2[xi], start=(jc == 0), stop=(jc == KC - 1))],
                [B_sq2[xi], B_cb], [B_ps[6 + t2]]))
            if jc4 == 3 and t2 == 1:
                w_issue()
        pend_ssq()
        for t2 in range(2):
            K.op(act, lambda e: e.activation(out=rs2[:, t2 * TT:(t2 + 1) * TT], in_=ps[6 + t2][:], func=AF.Ln,
                                             scale=1.0 / D, bias=EPS), [B_ps[6 + t2]], [B_rs2])
        K.op(act, lambda e: e.activation(out=rs2, in_=rs2, func=AF.Exp, scale=-0.5), [B_rs2], [B_rs2])
        if stage == "C" and hf == 1:
            for b in K.dbufs:
                if b.dcnt > 0:
                    sp.wait((b.dsem, b.dcnt))
            return nc
        K.barrier()
        yi = 0
        for ub in range(22):
            slot, B_slot = w_next()
            sl = slot[:].rearrange("p (k n) -> p k n", n=512)
            for pi in range(2):
                cp = 2 * ub + pi
                si = yi % 2
                yi += 1
                for gv in range(2):
                    j = cp + NFC * gv
                    y, B_yy = ybuf[si][gv], B_y[si][gv]
                    if hf == 0:
                        K.op(dve, lambda e: e.memset(y[:, 0:2], 0.0), [], [B_yy])
                    else:
                        K.op(act, lambda e: e.activation(out=y[:, 0:2], in_=halo[:, j, :], func=AF.Copy),
                             [B_halo], [B_yy])
                    for t2 in range(2):
                        bi = rot["mm"] % 4
                        rot["mm"] += 1
                        c0 = gv * 256 + pi * 128
                        K.mm([lambda e, kc=kc: e.matmul(ps[bi][:], sl[:, kc, c0:c0 + 128], h2p[:, kc, t2 * TT:(t2 + 1) * TT],
                                                        start=(kc == 0), stop=(kc == KC - 1)) for kc in range(KC)],
                             [B_slot, B_h2p], [B_ps[bi]])
                        K.op(dve, lambda e: e.tensor_tensor(out=y[:, 2 + t2 * TT:2 + (t2 + 1) * TT], in0=ps[bi][:],
                                                            in1=rs2[:, t2 * TT:(t2 + 1) * TT], op=ALU.mult),
                             [B_ps[bi], B_rs2], [B_yy])
                    if hf == 0:
                        K.op(act, lambda e: e.activation(out=halo[:, j, :], in_=y[:, HS:HS + 2], func=AF.Copy),
                             [B_yy], [B_halo])
                    a, B_a = (ag, B_ag) if gv == 0 else (av, B_av)
                    wc = lambda k: cf[:, CF_FCW + j * 3 + k:CF_FCW + j * 3 + k + 1]
                    K.op(act, lambda e: e.activation(out=a, in_=y[:, 2:2 + HS], func=AF.Identity, scale=wc(2),
                                                     bias=cf[:, CF_FCB + j:CF_FCB + j + 1]), [B_yy, B_cf], [B_a])
                    for k in (1, 0):
                        K.op(dve, lambda e, k=k: e.scalar_tensor_tensor(out=a, in0=y[:, k:k + HS], scalar=wc(k), in1=a,
                                                                        op0=ALU.mult, op1=ALU.add),
                             [B_yy, B_cf, B_a], [B_a])
                K.op(act, lambda e: e.activation(out=ag, in_=ag, func=AF.Silu), [B_ag], [B_ag])
                K.op(dve, lambda e: e.tensor_tensor(out=actT[:, cp, :], in0=ag, in1=av, op=ALU.mult),
                     [B_ag, B_av], [B_actT])
            w_issue()
        for jb2 in range(8):
            base = 0 if jb2 % 2 == 0 else 4
            for q4 in range(4):
                jc2, t2 = q4 // 2, q4 % 2
                jc = jb2 * 2 + jc2
                K.dma(sp, xr2[q4 % 2], x1d[jc * 128:(jc + 1) * 128, hoff + t2 * TT:hoff + (t2 + 1) * TT], B_xr2[q4 % 2], True) \
                    if q4 < 2 else None
            for kh in range(2):
                slot, B_slot = w_next()
                slv = slot[:, 0:22 * 256].rearrange("p (k n) -> p k n", n=256)
                for q4 in range(4):
                    jc2, t2 = q4 // 2, q4 % 2
                    bi = base + q4
                    K.mm([lambda e, k=k: e.matmul(ps[bi][:], slv[:, k, jc2 * 128:(jc2 + 1) * 128],
                                                  actT[:, kh * 22 + k, t2 * TT:(t2 + 1) * TT],
                                                  start=(kh == 0 and k == 0), stop=(kh == 1 and k == 21))
                          for k in range(22)], [B_slot, B_actT], [B_ps[bi]])
                w_issue()
            for q4 in range(4):
                jc2, t2 = q4 // 2, q4 % 2
                jc = jb2 * 2 + jc2
                bi = base + q4
                K.op(dve, lambda e: e.tensor_tensor(out=otb[q4 % 2], in0=ps[bi][:], in1=xr2[q4 % 2], op=ALU.add),
                     [B_ps[bi], B_xr2[q4 % 2]], [B_ot[q4 % 2]])
                K.dma(sp, outT[jc * 128:(jc + 1) * 128, hoff + t2 * TT:hoff + (t2 + 1) * TT], otb[q4 % 2], B_ot[q4 % 2], False)
                if q4 + 2 < 4:
                    jc2n, t2n = (q4 + 2) // 2, (q4 + 2) % 2
                    jcn = jb2 * 2 + jc2n
                    K.dma(sp, xr2[q4 % 2], x1d[jcn * 128:(jcn + 1) * 128, hoff + t2n * TT:hoff + (t2n + 1) * TT],
                          B_xr2[q4 % 2], True)

    for b in K.dbufs:
        if b.dcnt > 0:
            sp.wait((b.dsem, b.dcnt))
    return nc

def _consts():
    cbn = np.zeros((P, NCB), np.float32)
    i = np.arange(P)
    cbn[i, CB_ID + i] = 1.0
    cbn[:, CB_ONE:CB_ONE + 128] = 1.0
    for m in range(64):
        cbn[m + 64, CB_PSG + m] = -1.0
        cbn[m, CB_PSG + m + 64] = 1.0
    ik = np.arange(P)[:, None]
    iq = np.arange(P)[None, :]
    own = (ik <= iq).astype(np.float32)
    prev = (ik >= iq).astype(np.float32)
    cbn[:, CB_MOWN:CB_MOWN + 512] = np.tile(own, (1, 4))
    cbn[:, CB_MPREV:CB_MPREV + 512] = np.tile(prev, (1, 4))
    for T in range(4):
        m3 = (np.arange(P)[:, None] <= (32 * T + np.arange(32))[None, :]).astype(np.float32)
        cbn[:, CB_M3 + T * 512:CB_M3 + (T + 1) * 512] = np.repeat(m3, 16, axis=1)
    cbn[:, CB_MOWN4:CB_MOWN4 + 512] = np.repeat(own, 4, axis=1)
    cbn[:, CB_MPREV4:CB_MPREV4 + 512] = np.repeat(prev, 4, axis=1)
    half = 64
    inv_freq = (10000.0 ** (-np.arange(half, dtype=np.float32) / half)).astype(np.float32)
    ang = np.arange(S, dtype=np.float32)[None, :] * inv_freq[:, None]
    cos = np.cos(ang).astype(np.float32)
    sin = np.sin(ang).astype(np.float32)
    cs = np.concatenate([np.concatenate([cos, cos], 0), np.concatenate([sin, sin], 0)], 1)
    return cbn, np.ascontiguousarray(cs)


def _params(inp):
    cfn = np.zeros((P, NCF), np.float32)
    i = np.arange(P)
    cfn[:, CF_TRIU:CF_TRIU + 128] = (i[:, None] <= i[None, :]).astype(np.float32)
    cfn[:, CF_G1:CF_G1 + 16] = inp["norm_mix_g"][0].reshape(16, P).T
    cfn[:, CF_G2:CF_G2 + 16] = inp["norm_ffn_g"][0].reshape(16, P).T
    cfn[:, CF_QG] = inp["q_norm_g"][0]
    cfn[:, CF_KG] = inp["k_norm_g"][0]
    cw = inp["mlstm_conv_w"][0]
    cfn[:, CF_MCW:CF_MCW + 32] = cw.reshape(4, 8, P).transpose(2, 1, 0).reshape(P, 32)
    cfn[:, CF_MCB:CF_MCB + 8] = inp["mlstm_conv_b"][0].reshape(8, P).T
    cfn[:, CF_BG:CF_BG + 4] = inp["b_igate"][0][None, :]
    cfn[:, CF_BG + 4:CF_BG + 8] = inp["b_fgate"][0][None, :]
    cfn[:, CF_MNG:CF_MNG + 4] = inp["mlstm_norm_g"][0].reshape(4, P).T
    fw = inp["ffn_conv_w"][0]
    cfn[:, CF_FCW:CF_FCW + 264] = fw.reshape(3, 88, P).transpose(2, 1, 0).reshape(P, 264)
    cfn[:, CF_FCB:CF_FCB + 88] = inp["ffn_conv_b"][0].reshape(88, P).T
    return cfn


_NC_CACHE = {}


def run(inputs, stage="FULL", ncores=8):
    inp = {k: np.asarray(v) for k, v in inputs.items()}
    if stage not in _NC_CACHE:
        _NC_CACHE[stage] = build(stage)
    nc = _NC_CACHE[stage]
    cbn, cs = _consts()
    cfn = _params(inp)
    w_in = np.ascontiguousarray(inp["w_in"][0])
    w_out = np.ascontiguousarray(inp["w_out"][0])
    w_up = np.ascontiguousarray(inp["w_up"][0])
    w_down = np.ascontiguousarray(inp["w_down"][0])
    in_maps = []
    for b in range(ncores):
        in_maps.append({"xT": np.ascontiguousarray(inp["x"][b].T), "w_in": w_in, "w_out": w_out, "w_up": w_up,
                        "w_down": w_down, "cb": cbn, "cf": cfn, "cs": cs})
    res = run_bass_kernel_spmd(nc, in_maps, core_ids=list(range(ncores)))
    return res


def kernel(**inputs):
    res = run(inputs, "FULL", 8)
    out = np.stack([np.ascontiguousarray(r["outT"].T) for r in res.results], axis=0)
    return out.astype(np.float32)
```

```python
import os
import numpy as np
import concourse.bass as bass
import concourse.mybir as mybir
from concourse.bass_utils import run_bass_kernel_spmd

F32 = mybir.dt.float32
BF16 = mybir.dt.bfloat16
AF = mybir.ActivationFunctionType
ALU = mybir.AluOpType

S = 2048
D = 2048
P = 128
KC = 16
TT = 512
NT = 4
NHA = 12
NHM = 4
DFF = 5632
NFC = 44
INW = 6664
EPS = 1e-6
LNSC = float(np.log(128.0 ** -0.5))

CB_ID, CB_ONE, CB_PSG, CB_MOWN, CB_MPREV, CB_M3 = 0, 128, 256, 384, 896, 1408
CB_MOWN4 = 1408 + 4 * 512
CB_MPREV4 = CB_MOWN4 + 512
NCB = CB_MPREV4 + 512
CF_TRIU = 0
CF_G1 = 128
CF_G2 = CF_G1 + 16
CF_QG = CF_G2 + 16
CF_KG = CF_QG + 1
CF_MCW = CF_KG + 1
CF_MCB = CF_MCW + 32
CF_BG = CF_MCB + 8
CF_MNG = CF_BG + 8
CF_FCW = CF_MNG + 4
CF_FCB = CF_FCW + 264
NCF = CF_FCB + 88


class Eng:
    def __init__(self, nc, eng, name):
        self.e = eng
        self.name = name
        self.sem = nc.alloc_semaphore("s_" + name)
        self.cnt = 0
        self.waited = {}

    def wait(self, tok):
        if tok is None:
            return
        s, v = tok
        k = id(s)
        if self.waited.get(k, 0) >= v:
            return
        if self.name == "pe" and s is self.sem:
            return
        self.e.wait_ge(s, v)
        self.waited[k] = v

    def sig(self, ins):
        ins.then_inc(self.sem, 1)
        self.cnt += 1
        return (self.sem, self.cnt)


class Buf:
    def __init__(self, name, excl=False):
        self.name = name
        self.excl = excl
        self.w = None
        self.rs = {}
        self.dsem = None
        self.dcnt = 0

    def add_read(self, tok):
        self.rs[id(tok[0])] = tok

    def set_write(self, tok):
        self.w = tok
        self.rs = {}


class Ctx:
    def __init__(self):
        nc = bass.Bass("TRN2", target_bir_lowering=False)
        self.nc = nc
        self.pe = Eng(nc, nc.tensor, "pe")
        self.act = Eng(nc, nc.scalar, "act")
        self.dve = Eng(nc, nc.vector, "dve")
        self.pool = Eng(nc, nc.gpsimd, "pool")
        self.sp = Eng(nc, nc.sync, "sp")
        self.dbufs = []
        self.nbuf = 0

    def buf(self, name, excl=False):
        return Buf(name, excl)

    def deps(self, eng, reads, writes):
        for b in reads:
            eng.wait(b.w)
            if b.excl:
                for t in list(b.rs.values()):
                    eng.wait(t)
        for b in writes:
            eng.wait(b.w)
            for t in list(b.rs.values()):
                eng.wait(t)

    def mark(self, tok, reads, writes):
        for b in reads:
            if b.excl:
                b.set_write(tok)
            else:
                b.add_read(tok)
        for b in writes:
            b.set_write(tok)

    def op(self, eng, fn, reads=(), writes=()):
        self.deps(eng, reads, writes)
        ins = fn(eng.e)
        tok = eng.sig(ins)
        self.mark(tok, reads, writes)
        return tok

    def mm(self, fns, reads=(), writes=()):
        self.deps(self.pe, reads, writes)
        ins = None
        for f in fns:
            ins = f(self.pe.e)
        tok = self.pe.sig(ins)
        self.mark(tok, reads, writes)
        return tok

    def dma(self, eng, out, in_, sb, load, extra_reads=(), extra_writes=()):
        if sb.dsem is None:
            sb.dsem = self.nc.alloc_semaphore("d%d_%s" % (len(self.dbufs), sb.name))
            self.dbufs.append(sb)
        reads = list(extra_reads) + ([] if load else [sb])
        writes = list(extra_writes) + ([sb] if load else [])
        self.deps(eng, reads, writes)
        if not isinstance(out, (list, tuple)):
            out, in_ = [out], [in_]
        for o_, i_ in zip(out, in_):
            eng.e.dma_start(out=o_, in_=i_).then_inc(sb.dsem, 16)
            sb.dcnt += 16
        tok = (sb.dsem, sb.dcnt)
        self.mark(tok, reads, writes)
        return tok

    def barrier(self, with_pool=False):
        toks = []
        engs = [self.pe, self.act, self.dve, self.sp] + ([self.pool] if with_pool else [])
        for e in [self.pe, self.act, self.dve, self.pool]:
            if e.cnt > 0 and (with_pool or e is not self.pool):
                toks.append((e.sem, e.cnt))
        for b in self.dbufs:
            if b.dcnt > 0 and (with_pool or not b.name.startswith("wslot")):
                toks.append((b.dsem, b.dcnt))
        for e in engs:
            for t in toks:
                e.wait(t)


def build(stage="FULL"):
    K = Ctx()
    nc = K.nc
    pe, act, dve, pool, sp = K.pe, K.act, K.dve, K.pool, K.sp
    dbg = stage != "FULL"

    xT = nc.dram_tensor("xT", [D, S], F32, kind="ExternalInput").ap()
    w_in = nc.dram_tensor("w_in", [D, INW], F32, kind="ExternalInput").ap()
    w_out = nc.dram_tensor("w_out", [D, D], F32, kind="ExternalInput").ap()
    w_up = nc.dram_tensor("w_up", [D, 2 * DFF], F32, kind="ExternalInput").ap()
    w_down = nc.dram_tensor("w_down", [DFF, D], F32, kind="ExternalInput").ap()
    cbd = nc.dram_tensor("cb", [P, NCB], F32, kind="ExternalInput").ap()
    cfd = nc.dram_tensor("cf", [P, NCF], F32, kind="ExternalInput").ap()
    csd = nc.dram_tensor("cs", [P, 2 * S], F32, kind="ExternalInput").ap()
    outT = nc.dram_tensor("outT", [D, S], F32, kind="ExternalOutput").ap()
    mixd = nc.dram_tensor("mixd", [D, S], BF16, kind="ExternalOutput" if dbg else "Internal").ap()
    x1d = nc.dram_tensor("x1d", [D, S], F32, kind="ExternalOutput" if dbg else "Internal").ap()

    w_in_v = w_in.rearrange("(kc p) n -> p kc n", p=P)
    w_out_v = w_out.rearrange("(kc p) n -> p kc n", p=P)
    w_up_v = w_up.rearrange("(kc p) n -> p kc n", p=P)
    w_down_v = w_down.rearrange("(kc p) n -> p kc n", p=P)
    xT_v = xT.rearrange("(kc p) t -> p kc t", p=P)
    mixd_v = mixd.rearrange("(kc p) t -> p kc t", p=P)

    big1 = nc.alloc_sbuf_tensor("big1", [P, 32768], BF16)
    wslots = [nc.alloc_sbuf_tensor("wslot%d" % i, [P, 8192], BF16) for i in range(2)]
    B_ws = [K.buf("wslot%d" % i) for i in range(2)]
    cb = nc.alloc_sbuf_tensor("cbs", [P, NCB], BF16)
    cf = nc.alloc_sbuf_tensor("cfs", [P, NCF], F32)
    wg = nc.alloc_sbuf_tensor("wg", [P, KC, 8], BF16)
    ARENA_B = 95 * 1024
    arena = nc.alloc_sbuf_tensor("arena", [P, ARENA_B // 2], BF16)
    B_cb, B_cf, B_wg = K.buf("cb"), K.buf("cf"), K.buf("wg")

    def aview(boff, nelem, dt):
        assert boff % 4 == 0
        nb = nelem * (4 if dt == F32 else 2)
        assert boff + nb <= ARENA_B, (boff, nb)
        a = arena[:, boff // 2:(boff + nb) // 2]
        return a.bitcast(F32) if dt == F32 else a

    def b1view(boff, nelem, dt):
        nb = nelem * (4 if dt == F32 else 2)
        assert boff + nb <= 65536
        a = big1[:, boff // 2:(boff + nb) // 2]
        return a.bitcast(F32) if dt == F32 else a

    ps = [nc.alloc_psum_tensor("ps%d" % i, [P, 512], F32) for i in range(8)]
    B_ps = [K.buf("ps%d" % i, excl=True) for i in range(8)]

    ident = cb[:, CB_ID:CB_ID + 128]
    ones = cb[:, CB_ONE:CB_ONE + 128]
    psgn = cb[:, CB_PSG:CB_PSG + 128]
    mown = cb[:, CB_MOWN:CB_MOWN + 512]
    mprev = cb[:, CB_MPREV:CB_MPREV + 512]
    triu = cf[:, CF_TRIU:CF_TRIU + 128]

    K.dma(pool, cb[:], cbd, B_cb, True)
    K.dma(sp, cf[:], cfd, B_cf, True)
    K.dma(pool, wg[:], w_in_v[:, :, 6656:6664], B_wg, True)

    blocks = []
    for h in range(NHA):
        blocks.append([(lambda s, i=i: s[:].rearrange("p (k n) -> p k n", n=512)[:, :, i * 128:(i + 1) * 128],
                        w_in_v[:, :, c0:c0 + 128])
                       for i, c0 in enumerate([h * 128, 1536 + h * 128, 3072 + h * 128])])
    for h in range(NHM):
        blocks.append([(lambda s, i=i: s[:].rearrange("p (k n) -> p k n", n=512)[:, :, i * 128:(i + 1) * 128],
                        w_in_v[:, :, c0:c0 + 128])
                       for i, c0 in enumerate([4608 + h * 128, 5120 + h * 128, 5632 + h * 128, 6144 + h * 128])])
    for hf in range(2):
        for jb in range(4):
            blocks.append([(lambda s: s[:].rearrange("p (k n) -> p k n", n=512), w_out_v[:, :, jb * 512:(jb + 1) * 512])])
        for ub in range(22):
            blocks.append([(lambda s: s[:].rearrange("p (k n) -> p k n", n=512)[:, :, 0:256],
                            w_up_v[:, :, ub * 256:(ub + 1) * 256]),
                           (lambda s: s[:].rearrange("p (k n) -> p k n", n=512)[:, :, 256:512],
                            w_up_v[:, :, DFF + ub * 256:DFF + (ub + 1) * 256])])
        for jb2 in range(8):
            for kh in range(2):
                blocks.append([(lambda s: s[:, 0:22 * 256].rearrange("p (k n) -> p k n", n=256),
                                w_down_v[:, kh * 22:(kh + 1) * 22, jb2 * 256:(jb2 + 1) * 256])])
    wstate = {"issued": 0, "used": 0}

    def w_issue():
        i = wstate["issued"]
        if i >= len(blocks):
            return
        sl = i % 2
        K.dma(pool, [vf(wslots[sl]) for vf, src in blocks[i]], [src for vf, src in blocks[i]], B_ws[sl], True)
        wstate["issued"] = i + 1

    def w_next():
        i = wstate["used"]
        wstate["used"] = i + 1
        assert i < wstate["issued"]
        return wslots[i % 2], B_ws[i % 2]

    w_issue()
    w_issue()

    hT = big1[:].rearrange("p (k t) -> p k t", t=S)
    B_hT = K.buf("hT")

    TA = 256
    NTA = S // TA
    xs = [aview(i * 16384, KC * TA, F32).rearrange("p (k t) -> p k t", t=TA) for i in range(4)]
    B_xs = [K.buf("xs%d" % i) for i in range(4)]
    sqA = [aview(65536 + i * 8192, KC * TA, BF16).rearrange("p (k t) -> p k t", t=TA) for i in range(2)]
    B_sqA = [K.buf("sqA0"), K.buf("sqA1")]
    rsA = [aview(81920 + i * 1024, TA, F32) for i in range(2)]
    B_rsA = [K.buf("rsA0"), K.buf("rsA1")]
    for tt in range(3):
        K.dma(sp, xs[tt % 4], xT_v[:, :, tt * TA:(tt + 1) * TA], B_xs[tt % 4], True)
    for tt in range(NTA):
        x_t, B_x = xs[tt % 4], B_xs[tt % 4]
        sq_t, B_sq = sqA[tt % 2], B_sqA[tt % 2]
        rs_t, B_rs = rsA[tt % 2], B_rsA[tt % 2]
        pb = tt % 2
        if tt + 3 < NTA:
            K.dma(sp, xs[(tt + 3) % 4], xT_v[:, :, (tt + 3) * TA:(tt + 4) * TA], B_xs[(tt + 3) % 4], True)
        K.op(act, lambda e: e.activation(out=sq_t, in_=x_t, func=AF.Square), [B_x], [B_sq])
        K.mm([lambda e, kc=kc: e.matmul(ps[pb][:, 0:TA], ones, sq_t[:, kc, :], start=(kc == 0), stop=(kc == KC - 1))
              for kc in range(KC)], [B_sq, B_cb], [B_ps[pb]])
        K.op(act, lambda e: e.activation(out=rs_t, in_=ps[pb][:, 0:TA], func=AF.Ln, scale=1.0 / D, bias=EPS),
             [B_ps[pb]], [B_rs])
        K.op(act, lambda e: e.activation(out=rs_t, in_=rs_t, func=AF.Exp, scale=-0.5), [B_rs], [B_rs])
        for kc in range(KC):
            K.op(dve, lambda e, kc=kc: e.scalar_tensor_tensor(
                out=hT[:, kc, tt * TA:(tt + 1) * TA], in0=x_t[:, kc, :], scalar=cf[:, CF_G1 + kc:CF_G1 + kc + 1],
                in1=rs_t, op0=ALU.mult, op1=ALU.add if False else ALU.mult), [B_x, B_rs, B_cf], [B_hT])

    sp_final = []
    if stage == "A":
        sp_final.append(K.dma(sp, mixd_v, hT, B_hT, False))
        for t in sp_final:
            sp.wait(t)
        return nc

    K.barrier()

    psb = [ps[i].bitcast(BF16) for i in range(8)]
    cs = aview(0, 2 * S, F32)
    B_cs = K.buf("cs")
    K.dma(sp, cs, csd, B_cs, True)
    cos2 = cs[:, 0:S]
    sin2 = cs[:, S:2 * S]
    qTb = [aview(16384 + i * 4096, S, BF16) for i in range(2)]
    kTb = [aview(24576 + i * 4096, S, BF16) for i in range(2)]
    B_qT = [K.buf("qT0"), K.buf("qT1")]
    B_kT = [K.buf("kT0"), K.buf("kT1")]
    vT = aview(32768, S, BF16)
    B_vT = K.buf("vT")
    Vb = [[aview(36864 + (i * 3 + l) * 4096, S, BF16).rearrange("p (b d) -> p b d", d=128) for l in range(3)]
          for i in range(2)]
    B_V = [K.buf("V0"), K.buf("V1")]
    ptb = [aview(87040 + i * 1024, TT, BF16) for i in range(4)]
    B_pt = [K.buf("pt%d" % i) for i in range(4)]
    scr = []
    for i in range(2):
        o = 64512 + i * 8192
        scr.append(dict(sq=aview(o, TT, BF16), qg=aview(o + 1024, TT, BF16), rs=aview(o + 2048, TT, F32),
                        t1=aview(o + 4096, TT, F32), t2=aview(o + 6144, TT, F32),
                        B_sq=K.buf("sq%d" % i), B_qg=K.buf("qg%d" % i), B_rs=K.buf("rs%d" % i),
                        B_t1=K.buf("t1%d" % i), B_t2=K.buf("t2%d" % i)))
    recb = [aview(80896 + i * 2048, TT, F32) for i in range(2)]
    ostb = [aview(80896 + 4096 + i * 1024, TT, BF16) for i in range(2)]
    B_rec = [K.buf("rec0"), K.buf("rec1")]
    B_ost = [K.buf("ost0"), K.buf("ost1")]
    rot = {"mm": 0, "scr": 0, "S": 0, "pt": 0, "o": 0}

    def proj_group(h, slot, B_slot, part, tt):
        hb = h % 2
        sl = slot[:].rearrange("p (k n) -> p k n", n=512)
        bi = rot["mm"] % 2
        rot["mm"] += 1
        bank, B_bank = ps[bi], B_ps[bi]
        K.mm([lambda e, kc=kc: e.matmul(bank[:], sl[:, kc, part * 128:(part + 1) * 128],
                                        hT[:, kc, tt * TT:(tt + 1) * TT], start=(kc == 0), stop=(kc == KC - 1))
              for kc in range(KC)], [B_slot, B_hT], [B_bank])
        tsl = slice(tt * TT, (tt + 1) * TT)
        if part == 2:
            K.op(act, lambda e: e.activation(out=vT[:, tsl], in_=bank[:], func=AF.Copy), [B_bank], [B_vT])
            return None
        sc = scr[rot["scr"] % 2]
        rot["scr"] += 1
        gcol = CF_QG if part == 0 else CF_KG
        dst, B_dst = (qTb[hb], B_qT[hb]) if part == 0 else (kTb[hb], B_kT[hb])
        K.op(act, lambda e: e.activation(out=sc["sq"], in_=bank[:], func=AF.Square), [B_bank], [sc["B_sq"]])
        K.op(act, lambda e: e.activation(out=sc["qg"], in_=bank[:], func=AF.Copy, scale=cf[:, gcol:gcol + 1]),
             [B_bank, B_cf], [sc["B_qg"]])

        def stage2():
            K.mm([lambda e: e.matmul(ps[2][:], ones, sc["sq"], start=True, stop=True)], [sc["B_sq"], B_cb], [B_ps[2]])
            K.mm([lambda e: e.matmul(ps[3][:], psgn, sc["qg"], start=True, stop=True)], [sc["B_qg"], B_cb], [B_ps[3]])
            if part == 0:
                K.op(act, lambda e: e.activation(out=sc["rs"], in_=ps[2][:], func=AF.Ln, scale=1.0, bias=128.0 * EPS),
                     [B_ps[2]], [sc["B_rs"]])
            else:
                K.op(act, lambda e: e.activation(out=sc["rs"], in_=ps[2][:], func=AF.Ln, scale=1.0 / 128.0, bias=EPS),
                     [B_ps[2]], [sc["B_rs"]])
            K.op(act, lambda e: e.activation(out=sc["rs"], in_=sc["rs"], func=AF.Exp, scale=-0.5),
                 [sc["B_rs"]], [sc["B_rs"]])
            K.op(dve, lambda e: e.tensor_tensor(out=sc["t2"], in0=ps[3][:], in1=sin2[:, tsl], op=ALU.mult),
                 [B_ps[3], B_cs], [sc["B_t2"]])
            K.op(dve, lambda e: e.tensor_tensor(out=sc["t1"], in0=sc["qg"], in1=cos2[:, tsl], op=ALU.mult),
                 [sc["B_qg"], B_cs], [sc["B_t1"]])
            K.op(dve, lambda e: e.tensor_tensor(out=sc["t1"], in0=sc["t1"], in1=sc["t2"], op=ALU.add),
                 [sc["B_t1"], sc["B_t2"]], [sc["B_t1"]])
            K.op(dve, lambda e: e.tensor_tensor(out=dst[:, tsl], in0=sc["t1"], in1=sc["rs"], op=ALU.mult),
                 [sc["B_t1"], sc["B_rs"]], [B_dst])
        return stage2

    def v_layouts(h):
        hb = h % 2
        V1, V2, V3 = Vb[hb]
        srcs = []
        for kb in range(16):
            srcs.append((0, kb, vT[:, kb * 128:(kb + 1) * 128]))
        for r in range(4):
            for nb in range(4):
                srcs.append((1, r * 4 + nb, vT[:, nb * 512 + r:(nb + 1) * 512:4]))
        for r in range(16):
            srcs.append((2, r, vT[:, r:S:16]))
        for g in range(6):
            bi = 2 + (g % 2)
            bank = psb[bi][:].rearrange("p (b d) -> p b d", d=128)
            grp = srcs[g * 8:(g + 1) * 8]
            K.mm([lambda e, j=j, src=src: e.transpose(bank[:, j, :], src, ident)
                  for j, (_, _, src) in enumerate(grp)], [B_vT, B_cb], [B_ps[bi]])
            l = grp[0][0]
            b0 = grp[0][1]
            dstv = Vb[hb][l][:, b0:b0 + 8, :]
            K.op(act, lambda e: e.activation(out=dstv, in_=bank, func=AF.Copy), [B_ps[bi]], [B_V[hb]])
            yield

    def attn_tile(h, T):
        hb = h % 2
        qT_, kT_ = qTb[hb], kTb[hb]
        V1, V2, V3 = Vb[hb]
        num, den = ps[6], ps[7]
        num4 = num[:].rearrange("p (j r) -> p r j", r=4)
        num16 = num[:].rearrange("p (j r) -> p r j", r=16)
        den4 = den[:].rearrange("p (j r) -> p r j", r=4)
        den16 = den[:].rearrange("p (j r) -> p r j", r=16)
        t0 = T * TT
        banks = []
        def v4(ap_):
            return ap_.rearrange("p (j r) -> p r j", r=4)

        def v16(ap_):
            return ap_.rearrange("p (j r) -> p r j", r=16)
        fl = lambda i: (lambda x: x[:, 128 * i:128 * (i + 1)])
        r4 = lambda r: (lambda x: v4(x[:, 0:TT])[:, r, :])
        r16 = lambda r: (lambda x: v16(x[:, 0:TT])[:, r, :])
        own1 = [(fl(i), kT_[:, (4 * T + i) * 128:(4 * T + i + 1) * 128],
                 qT_[:, (4 * T + i) * 128:(4 * T + i + 1) * 128]) for i in range(4)]
        pv_own1 = [(fl(i), V1[:, 4 * T + i, :]) for i in range(4)]
        banks.append(("own1", 0, own1, mown, pv_own1))
        i0 = 1 if T == 0 else 0
        prev1 = [(fl(i), kT_[:, (4 * T + i - 1) * 128:(4 * T + i) * 128],
                  qT_[:, (4 * T + i) * 128:(4 * T + i + 1) * 128]) for i in range(i0, 4)]
        pv_prev1 = [(fl(i), V1[:, 4 * T + i - 1, :]) for i in range(i0, 4)]
        banks.append(("prev1", 128 * i0, prev1, mprev, pv_prev1))
        own2 = [(r4(r), kT_[:, t0 + r:t0 + TT:4], qT_[:, t0 + r:t0 + TT:4]) for r in range(4)]
        pv_own2 = [(r4(r), V2[:, r * 4 + T, :]) for r in range(4)]
        banks.append(("own2", 0, own2, cb[:, CB_MOWN4:CB_MOWN4 + 512], pv_own2))
        if T >= 1:
            prev2 = [(r4(r), kT_[:, t0 - TT + r:t0:4], qT_[:, t0 + r:t0 + TT:4]) for r in range(4)]
            pv_prev2 = [(r4(r), V2[:, r * 4 + T - 1, :]) for r in range(4)]
            banks.append(("prev2", 0, prev2, cb[:, CB_MPREV4:CB_MPREV4 + 512], pv_prev2))
        b3 = [(r16(r), kT_[:, r:S:16], qT_[:, t0 + r:t0 + TT:16]) for r in range(16)]
        pv_b3 = [(r16(r), V3[:, r, :]) for r in range(16)]
        banks.append(("b3", 0, b3, cb[:, CB_M3 + T * 512:CB_M3 + (T + 1) * 512], pv_b3))
        return banks

    def attn_head(h):
        hb = h % 2
        num, den = ps[6], ps[7]
        den4 = den[:].rearrange("p (j r) -> p r j", r=4)
        den16 = den[:].rearrange("p (j r) -> p r j", r=16)
        allb = []
        for T in range(NT):
            bl = attn_tile(h, T)
            for k, bk in enumerate(bl):
                allb.append((T, k == 0, k == len(bl) - 1, bk))
        pend = []

        def do_pv(item):
            T, first, last, (name, c0, smm, mask, pvl), pt, B_p = item
            fns = []
            for k, (colf, lhsT) in enumerate(pvl):
                st = first and k == 0
                fns.append(lambda e, colf=colf, lhsT=lhsT, st=st: e.matmul(
                    colf(num), lhsT, colf(pt), start=st, stop=False, skip_group_check=True))
            fns.append(lambda e, st=first: e.matmul(den[:, c0:TT], ones, pt[:, c0:TT], start=st, stop=False,
                                                    skip_group_check=True))
            K.mm(fns, [B_p, B_V[hb], B_cb], [B_ps[6], B_ps[7]])
            if last:
                t0 = T * TT
                oi = rot["o"] % 2
                rot["o"] += 1
                K.op(act, lambda e: e.activation(out=recb[oi], in_=den[:], func=AF.Ln), [B_ps[7]], [B_rec[oi]])
                K.op(act, lambda e: e.activation(out=recb[oi], in_=recb[oi], func=AF.Exp, scale=-1.0),
                     [B_rec[oi]], [B_rec[oi]])
                K.op(dve, lambda e: e.tensor_tensor(out=ostb[oi], in0=num[:], in1=recb[oi], op=ALU.mult),
                     [B_ps[6], B_rec[oi]], [B_ost[oi]])
                K.dma(sp, mixd[h * 128:(h + 1) * 128, t0:t0 + TT], ostb[oi], B_ost[oi], False)

        for (T, first, last, bk) in allb:
            (name, c0, smm, mask, pvl) = bk
            si = 4 + rot["S"] % 2
            rot["S"] += 1
            pi = rot["pt"] % 4
            rot["pt"] += 1
            Sb, B_S = ps[si], B_ps[si]
            pt, B_p = ptb[pi], B_pt[pi]
            K.mm([lambda e, a=a: e.matmul(a[0](Sb), a[1], a[2], start=True, stop=True)
                  for a in smm], [B_qT[hb], B_kT[hb]], [B_S])
            K.op(act, lambda e: e.activation(out=pt[:, c0:TT], in_=Sb[:, c0:TT], func=AF.Exp), [B_S], [B_p])
            K.op(dve, lambda e: e.tensor_tensor(out=pt[:, c0:TT], in0=pt[:, c0:TT], in1=mask[:, c0:TT], op=ALU.mult),
                 [B_p, B_cb], [B_p])
            pend.append((T, first, last, bk, pt, B_p))
            if len(pend) > 2:
                do_pv(pend.pop(0))
            yield
        while pend:
            do_pv(pend.pop(0))
            yield

    nheads = NHA if stage != "B1" else 2
    for h in range(nheads + 1):
        yth = attn_head(h - 1) if h >= 1 else None

        def ystep(n):
            nonlocal yth
            for _ in range(n):
                if yth is None:
                    return
                try:
                    next(yth)
                except StopIteration:
                    yth = None
        if h < nheads:
            slot, B_slot = w_next()
            prev2 = None
            for g in range(12):
                st2 = proj_group(h, slot, B_slot, g // 4, g % 4)
                if prev2 is not None:
                    prev2()
                prev2 = st2
                ystep(2)
            w_issue()
            for _ in v_layouts(h):
                ystep(1)
        ystep(1000)

    if stage in ("B", "B1"):
        for b in K.dbufs:
            if b.dcnt > 0:
                sp.wait((b.dsem, b.dcnt))
        return nc
    K.barrier()

    def bc_ap(base, dims):
        return bass.AP(base.tensor, base.offset, [list(base.ap[0])] + [list(d) for d in dims])

    XPW = 2056
    xp = [aview(i * XPW * 4, XPW, F32) for i in range(2)]
    B_xp = [K.buf("xpq"), K.buf("xpk")]
    accv = aview(2 * XPW * 4, S, F32)
    B_acc = K.buf("acc")
    o0 = 24640
    mqT = aview(o0, 4 * S, BF16).rearrange("p (h t) -> p h t", t=S)
    mkT = aview(o0 + 16384, 4 * S, BF16).rearrange("p (h t) -> p h t", t=S)
    mvt = aview(o0 + 32768, 16 * 512, BF16).rearrange("p (c n) -> p c n", n=512)
    sgo = aview(o0 + 49152, 4 * S, BF16).rearrange("p (h t) -> p h t", t=S)
    B_mq, B_mk, B_mv, B_sgo = K.buf("mqT"), K.buf("mkT"), K.buf("mvt"), K.buf("sgo")
    o1 = o0 + 65536
    gt = aview(o1, 128, F32).rearrange("p (c g) -> p c g", g=8)
    e1 = aview(o1 + 512, 64, F32).rearrange("p (c g) -> p c g", g=4)
    lf = aview(o1 + 768, 64, F32).rearrange("p (c g) -> p c g", g=4)
    ib = aview(o1 + 1024, 64, F32).rearrange("p (c g) -> p c g", g=4)
    ebias = aview(o1 + 1280, 64, F32).rearrange("p (c g) -> p c g", g=4)
    B_gt, B_e1, B_lf, B_ib, B_eb = [K.buf(n) for n in ("gt", "e1", "lf", "ib", "ebias")]
    vTm = aview(o1 + 1792, S, BF16)
    B_vTm = K.buf("vTm")

    for i in range(2):
        K.op(dve, lambda e, i=i: e.memset(xp[i][:, 0:3], 0.0), [], [B_xp[i]])

    for c in range(16):
        K.mm([lambda e, kc=kc, c=c: e.matmul(ps[7][:, c * 8:(c + 1) * 8], hT[:, kc, c * 128:(c + 1) * 128], wg[:, kc, :],
                                             start=(kc == 0), stop=(kc == KC - 1)) for kc in range(KC)],
             [B_hT, B_wg], [B_ps[7]])
    bgb = bc_ap(cf[:, CF_BG:CF_BG + 8], [[0, 16], [1, 8]])
    K.op(dve, lambda e: e.tensor_tensor(out=gt, in0=ps[7][:, 0:128].rearrange("p (c g) -> p c g", g=8), in1=bgb,
                                        op=ALU.add), [B_ps[7], B_cf], [B_gt])
    K.op(act, lambda e: e.activation(out=e1, in_=gt[:, :, 4:8], func=AF.Exp, scale=-1.0), [B_gt], [B_e1])
    K.op(act, lambda e: e.activation(out=e1, in_=e1, func=AF.Ln, bias=1.0, scale=1.0), [B_e1], [B_e1])
    K.op(dve, lambda e: e.tensor_scalar(out=lf, in0=e1, scalar1=-1.0, scalar2=None, op0=ALU.mult), [B_e1], [B_lf])
    K.mm([lambda e, c=c: e.matmul(ps[6][:, c * 4:(c + 1) * 4], triu, lf[:, c, :], start=True, stop=True)
          for c in range(16)], [B_lf, B_cf], [B_ps[6]])
    K.op(dve, lambda e: e.scalar_tensor_tensor(out=ib, in0=gt[:, :, 0:4], scalar=LNSC,
                                               in1=ps[6][:, 0:64].rearrange("p (c g) -> p c g", g=4),
                                               op0=ALU.add, op1=ALU.subtract), [B_gt, B_ps[6]], [B_ib])
    K.op(act, lambda e: e.activation(out=ebias, in_=ib, func=AF.Exp), [B_ib], [B_eb])

    for hm in range(NHM):
        slot, B_slot = w_next()
        sl = slot[:].rearrange("p (k n) -> p k n", n=512)
        for part in range(2):
            for tt in range(NT):
                bi = rot["mm"] % 2
                rot["mm"] += 1
                K.mm([lambda e, kc=kc: e.matmul(ps[bi][:], sl[:, kc, part * 128:(part + 1) * 128],
                                                hT[:, kc, tt * TT:(tt + 1) * TT], start=(kc == 0), stop=(kc == KC - 1))
                      for kc in range(KC)], [B_slot, B_hT], [B_ps[bi]])
                K.op(act, lambda e: e.activation(out=xp[part][:, 3 + tt * TT:3 + (tt + 1) * TT], in_=ps[bi][:],
                                                 func=AF.Copy), [B_ps[bi]], [B_xp[part]])
            j = part * 4 + hm
            wc = lambda k: cf[:, CF_MCW + j * 4 + k:CF_MCW + j * 4 + k + 1]
            K.op(dve, lambda e: e.tensor_scalar(out=accv, in0=xp[part][:, 3:3 + S], scalar1=wc(3),
                                                scalar2=cf[:, CF_MCB + j:CF_MCB + j + 1], op0=ALU.mult, op1=ALU.add),
                 [B_xp[part], B_cf], [B_acc])
            for k in (2, 1, 0):
                K.op(dve, lambda e, k=k: e.scalar_tensor_tensor(out=accv, in0=xp[part][:, k:k + S], scalar=wc(k),
                                                                in1=accv, op0=ALU.mult, op1=ALU.add),
                     [B_xp[part], B_cf, B_acc], [B_acc])
            dstT, B_d = (mqT, B_mq) if part == 0 else (mkT, B_mk)
            K.op(act, lambda e: e.activation(out=dstT[:, hm, :], in_=accv, func=AF.Silu), [B_acc], [B_d])
        for tt in range(NT):
            bi = rot["mm"] % 2
            rot["mm"] += 1
            K.mm([lambda e, kc=kc: e.matmul(ps[bi][:], sl[:, kc, 256:384], hT[:, kc, tt * TT:(tt + 1) * TT],
                                            start=(kc == 0), stop=(kc == KC - 1)) for kc in range(KC)],
                 [B_slot, B_hT], [B_ps[bi]])
            K.op(act, lambda e: e.activation(out=vTm[:, tt * TT:(tt + 1) * TT], in_=ps[bi][:], func=AF.Copy),
                 [B_ps[bi]], [B_vTm])
        for g in range(2):
            bank = psb[2 + g][:].rearrange("p (b d) -> p b d", d=128)
            K.mm([lambda e, j=j: e.transpose(bank[:, j, :], vTm[:, (g * 8 + j) * 128:(g * 8 + j + 1) * 128], ident)
                  for j in range(8)], [B_vTm, B_cb], [B_ps[2 + g]])
            K.op(act, lambda e: e.activation(out=mvt[:, g * 8:(g + 1) * 8, hm * 128:(hm + 1) * 128], in_=bank,
                                             func=AF.Copy), [B_ps[2 + g]], [B_mv])
        for tt in range(NT):
            bi = rot["mm"] % 2
            rot["mm"] += 1
            K.mm([lambda e, kc=kc: e.matmul(ps[bi][:], sl[:, kc, 384:512], hT[:, kc, tt * TT:(tt + 1) * TT],
                                            start=(kc == 0), stop=(kc == KC - 1)) for kc in range(KC)],
                 [B_slot, B_hT], [B_ps[bi]])
            K.op(act, lambda e: e.activation(out=sgo[:, hm, tt * TT:(tt + 1) * TT], in_=ps[bi][:], func=AF.Sigmoid),
                 [B_ps[bi]], [B_sgo])
        w_issue()

    K.barrier()
    b1o = [0]

    def b1a(nelem, dt):
        nb = nelem * (4 if dt == F32 else 2)
        o = b1o[0]
        b1o[0] = o + ((nb + 31) // 32) * 32
        return b1view(o, nelem, dt)

    def v4(dt=F32):
        return b1a(512, dt).rearrange("p (h s) -> p h s", s=128)
    lfrep = v4()
    Et3 = [v4() for _ in range(3)]
    dm = v4()
    st2 = [b1a(512, BF16) for _ in range(2)]
    qs2 = [v4(BF16) for _ in range(2)]
    dn2 = [b1a(512, F32) for _ in range(4)]
    nS2 = [b1a(512, F32) for _ in range(4)]
    hg2 = [v4() for _ in range(2)]
    sqh = b1a(512, BF16)
    rsm = v4()
    hmst = v4(BF16)
    wk2 = [b1a(4, F32) for _ in range(2)]
    kw2 = [v4(BF16) for _ in range(2)]
    Cst = b1a(1024, F32).rearrange("p (h n) -> p h n", n=256)
    Cbf = b1a(1024, BF16).rearrange("p (h n) -> p h n", n=256)
    (B_lfrep, B_dm, B_sqh, B_rsm, B_hmst, B_Cst, B_Cbf) = [
        K.buf(n) for n in ("lfrep", "dm", "sqh", "rsm", "hmst", "Cst", "Cbf")]
    B_E3 = [K.buf("E%d" % i) for i in range(3)]
    B_st2 = [K.buf("st0"), K.buf("st1")]
    B_qs2 = [K.buf("qs0"), K.buf("qs1")]
    B_dn2 = [K.buf("dn%d" % i) for i in range(4)]
    B_nS2 = [K.buf("nS%d" % i) for i in range(4)]
    B_hg2 = [K.buf("hg0"), K.buf("hg1")]
    B_wk2 = [K.buf("wk0"), K.buf("wk1")]
    B_kw2 = [K.buf("kw0"), K.buf("kw1")]
    mown128 = cb[:, CB_MOWN:CB_MOWN + 128]
    mixm = mixd[1536:2048, :].rearrange("(h p) t -> p h t", p=P)
    ktp = psb[5][:, 0:512].rearrange("p (h d) -> p h d", d=128)
    NCH = 16

    def st_P1(c):
        csl = slice(c * 128, (c + 1) * 128)
        Et, B_E = Et3[c % 3], B_E3[c % 3]
        K.op(dve, lambda e: e.tensor_copy(out=lfrep, in_=bc_ap(lf[:, c, :], [[1, 4], [0, 128]])), [B_lf], [B_lfrep])
        K.mm([lambda e, h=h: e.matmul(ps[0][:, h * 128:(h + 1) * 128], lfrep[:, h, :], triu, start=True, stop=True)
              for h in range(4)], [B_lfrep, B_cf], [B_ps[0]])
        K.op(act, lambda e: e.activation(out=Et, in_=ps[0][:].rearrange("p (h s) -> p h s", s=128), func=AF.Exp),
             [B_ps[0]], [B_E])

    def st_P1b(c):
        csl = slice(c * 128, (c + 1) * 128)
        K.mm([lambda e, h=h: e.matmul(ps[1][:, h * 128:(h + 1) * 128], mkT[:, h, csl], mqT[:, h, csl],
                                      start=True, stop=True) for h in range(4)], [B_mk, B_mq], [B_ps[1]])
        if c < NCH - 1:
            K.mm([lambda e, h=h: e.transpose(ktp[:, h, :], mkT[:, h, csl], ident) for h in range(4)],
                 [B_mk, B_cb], [B_ps[5]])

    def st_P2(c):
        cb_ = c % 2
        csl = slice(c * 128, (c + 1) * 128)
        Et, B_E = Et3[c % 3], B_E3[c % 3]
        K.op(dve, lambda e: e.tensor_tensor(out=dm[:].rearrange("p h s -> p (h s)"), in0=Et[:].rearrange("p h s -> p (h s)"),
                                            in1=mown, op=ALU.mult), [B_E, B_cb], [B_dm])
        K.op(dve, lambda e: e.tensor_tensor(out=dm, in0=dm, in1=bc_ap(ebias[:, c, :], [[1, 4], [0, 128]]), op=ALU.mult),
             [B_dm, B_eb], [B_dm])
        K.op(dve, lambda e: e.tensor_tensor(out=st2[cb_], in0=ps[1][:], in1=dm[:].rearrange("p h s -> p (h s)"),
                                            op=ALU.mult), [B_ps[1], B_dm], [B_st2[cb_]])
        K.op(dve, lambda e: e.tensor_tensor(out=qs2[cb_], in0=mqT[:, :, csl], in1=Et, op=ALU.mult),
             [B_mq, B_E], [B_qs2[cb_]])
        if c < NCH - 1:
            K.op(dve, lambda e: e.tensor_tensor(out=wk2[cb_], in0=ebias[:, c, :], in1=Et[:, :, 127], op=ALU.mult),
                 [B_eb, B_E], [B_wk2[cb_]])
            for h in range(4):
                K.op(act, lambda e, h=h: e.activation(out=kw2[cb_][:, h, :], in_=ktp[:, h, :], func=AF.Copy,
                                                      scale=wk2[cb_][:, h:h + 1]), [B_ps[5], B_wk2[cb_]], [B_kw2[cb_]])

    def st_R(c):
        cb_ = c % 2
        Et, B_E = Et3[c % 3], B_E3[c % 3]
        st_, qs_, kw_ = st2[cb_], qs2[cb_], kw2[cb_]
        if c < NCH - 1:
            fns = []
            for h in range(4):
                bk = ps[6 + h // 2]
                o = (h % 2) * 256
                fns.append(lambda e, h=h, bk=bk, o=o: e.matmul(bk[:, o:o + 128], kw_[:, h, :], mvt[:, c, h * 128:(h + 1) * 128],
                                                               start=True, stop=True))
                fns.append(lambda e, h=h, bk=bk, o=o: e.matmul(bk[:, o + 128:o + 256], kw_[:, h, :], ones, start=True, stop=True))
            K.mm(fns, [B_kw2[cb_], B_mv, B_cb], [B_ps[6], B_ps[7]])
        fns = []
        for h in range(4):
            hs = slice(h * 128, (h + 1) * 128)
            if c > 0:
                fns.append(lambda e, h=h, hs=hs: e.matmul(ps[2][:, hs], Cbf[:, h, 0:128], qs_[:, h, :], start=True, stop=False))
            fns.append(lambda e, h=h, hs=hs: e.matmul(ps[2][:, hs], mvt[:, c, hs], st_[:, hs], start=(c == 0), stop=True))
            if c > 0:
                fns.append(lambda e, h=h, hs=hs: e.matmul(ps[3][:, hs], Cbf[:, h, 128:256], qs_[:, h, :], start=True, stop=False))
            fns.append(lambda e, h=h, hs=hs: e.matmul(ps[3][:, hs], ones, st_[:, hs], start=(c == 0), stop=True))
        K.mm(fns, [B_Cbf, B_qs2[cb_], B_mv, B_st2[cb_], B_cb], [B_ps[2], B_ps[3]])
        if c < NCH - 1:
            for h in range(4):
                bk = ps[6 + h // 2]
                o = (h % 2) * 256
                if c == 0:
                    K.op(dve, lambda e, h=h, bk=bk, o=o: e.tensor_copy(out=Cst[:, h, :], in_=bk[:, o:o + 256]),
                         [B_ps[6 + h // 2]], [B_Cst])
                else:
                    K.op(dve, lambda e, h=h, bk=bk, o=o: e.scalar_tensor_tensor(
                        out=Cst[:, h, :], in0=Cst[:, h, :], scalar=Et[:, h, 127:128], in1=bk[:, o:o + 256],
                        op0=ALU.mult, op1=ALU.add), [B_Cst, B_E, B_ps[6 + h // 2]], [B_Cst])
            K.op(act, lambda e: e.activation(out=Cbf, in_=Cst, func=AF.Copy), [B_Cst], [B_Cbf])
        K.op(act, lambda e: e.activation(out=dn2[c % 4], in_=ps[3][:], func=AF.Abs), [B_ps[3]], [B_dn2[c % 4]])
        K.op(act, lambda e: e.activation(out=nS2[c % 4], in_=ps[2][:], func=AF.Copy), [B_ps[2]], [B_nS2[c % 4]])

    def st_T1a(c):
        cb_ = c % 4
        dn = dn2[cb_]
        K.op(dve, lambda e: e.tensor_scalar(out=dn, in0=dn, scalar1=1.0, scalar2=None, op0=ALU.max), [B_dn2[cb_]], [B_dn2[cb_]])
        K.op(act, lambda e: e.activation(out=dn, in_=dn, func=AF.Ln), [B_dn2[cb_]], [B_dn2[cb_]])
        K.op(act, lambda e: e.activation(out=dn, in_=dn, func=AF.Exp, scale=-1.0), [B_dn2[cb_]], [B_dn2[cb_]])

    def st_T1b(c):
        cb_ = c % 4
        csl = slice(c * 128, (c + 1) * 128)
        dn, hg = dn2[cb_], hg2[c % 2]
        K.op(dve, lambda e: e.tensor_tensor(out=hg[:].rearrange("p h s -> p (h s)"), in0=nS2[cb_], in1=dn, op=ALU.mult),
             [B_nS2[cb_], B_dn2[cb_]], [B_hg2[c % 2]])
        K.op(dve, lambda e: e.tensor_tensor(out=hg, in0=hg, in1=sgo[:, :, csl], op=ALU.mult), [B_hg2[c % 2], B_sgo], [B_hg2[c % 2]])
        K.op(act, lambda e: e.activation(out=sqh, in_=hg[:].rearrange("p h s -> p (h s)"), func=AF.Square),
             [B_hg2[c % 2]], [B_sqh])

    def st_T2a(c):
        K.mm([lambda e: e.matmul(ps[4][:], ones, sqh, start=True, stop=True)], [B_sqh, B_cb], [B_ps[4]])
        K.op(act, lambda e: e.activation(out=rsm[:].rearrange("p h s -> p (h s)"), in_=ps[4][:], func=AF.Ln,
                                         scale=1.0 / 128.0, bias=EPS), [B_ps[4]], [B_rsm])
        K.op(act, lambda e: e.activation(out=rsm, in_=rsm, func=AF.Exp, scale=-0.5), [B_rsm], [B_rsm])

    def st_T2b(c):
        cb_ = c % 2
        csl = slice(c * 128, (c + 1) * 128)
        hg = hg2[cb_]
        K.op(dve, lambda e: e.tensor_tensor(out=hg, in0=hg, in1=rsm, op=ALU.mult), [B_hg2[cb_], B_rsm], [B_hg2[cb_]])
        K.op(dve, lambda e: e.tensor_tensor(out=hmst, in0=hg, in1=bc_ap(cf[:, CF_MNG:CF_MNG + 4], [[1, 4], [0, 128]]),
                                            op=ALU.mult), [B_hg2[cb_], B_cf], [B_hmst])
        K.dma(sp, mixm[:, :, csl], hmst, B_hmst, False)

    def ok(c):
        return 0 <= c < NCH
    for it in range(-2, NCH + 4):
        if ok(it):
            st_R(it)
        if ok(it - 3):
            st_T2a(it - 3)
        if ok(it + 2):
            st_P1(it + 2)
        if ok(it + 1):
            st_P2(it + 1)
        if ok(it - 3):
            st_T2b(it - 3)
        if ok(it + 2):
            st_P1b(it + 2)
        if ok(it - 1):
            st_T1a(it - 1)
        if ok(it - 2):
            st_T1b(it - 2)

    if stage == "B2":
        for b in K.dbufs:
            if b.dcnt > 0:
                sp.wait((b.dsem, b.dcnt))
        return nc

    HS = 1024
    mixh = b1view(0, KC * HS, BF16).rearrange("p (k t) -> p k t", t=HS)
    h2p = b1view(32768, KC * HS, BF16).rearrange("p (k t) -> p k t", t=HS)
    B_mixh, B_h2p = K.buf("mixh"), K.buf("h2p")
    B_mixh2 = [K.buf("mixh_a"), K.buf("mixh_b")]
    actT = aview(0, NFC * HS, BF16).rearrange("p (c t) -> p c t", t=HS)
    B_actT = K.buf("actT")
    rs2 = aview(90112, HS, F32)
    halo = aview(94208, 176, F32).rearrange("p (j w) -> p j w", w=2)
    B_rs2, B_halo = K.buf("rs2"), K.buf("halo")
    xr = [aview(i * 2048, TT, F32) for i in range(4)]
    x1t = [aview(8192 + i * 2048, TT, F32) for i in range(2)]
    sq2 = [aview(12288 + i * 1024, TT, BF16) for i in range(2)]
    B_xr = [K.buf("xr%d" % i) for i in range(4)]
    B_x1t = [K.buf("x1t%d" % i) for i in range(2)]
    B_sq2 = [K.buf("sq2%d" % i) for i in range(2)]
    YW = 1026
    ybuf = [[b1view((si * 2 + gv) * YW * 4, YW, F32) for gv in range(2)] for si in range(2)]
    B_y = [[K.buf("y%d%d" % (si, gv)) for gv in range(2)] for si in range(2)]
    ag = b1view(4 * YW * 4, HS, F32)
    av = b1view(4 * YW * 4 + 4096, HS, F32)
    B_ag, B_av = K.buf("ag"), K.buf("av")
    xr2 = [b1view(4 * YW * 4 + 8192 + i * 2048, TT, F32) for i in range(2)]
    B_xr2 = [K.buf("xr20"), K.buf("xr21")]
    otb = [ag[:, 0:TT], av[:, 0:TT]]
    B_ot = [B_ag, B_av]

    for hf in range(2):
        hoff = hf * HS
        K.barrier()
        for t2_ in range(2):
            K.dma(sp, mixh[:, :, t2_ * TT:(t2_ + 1) * TT], mixd_v[:, :, hoff + t2_ * TT:hoff + (t2_ + 1) * TT],
                  B_mixh2[t2_], True)
        steps = [(jb, jc4, t2) for jb in range(4) for jc4 in range(4) for t2 in range(2)]

        def xload(i):
            jb, jc4, t2 = steps[i]
            jc = jb * 4 + jc4
            K.dma(sp, xr[i % 4], xT[jc * 128:(jc + 1) * 128, hoff + t2 * TT:hoff + (t2 + 1) * TT], B_xr[i % 4], True)
        xload(0)
        xload(1)
        slot = None
        pend_ssq = None
        for i, (jb, jc4, t2) in enumerate(steps):
            jc = jb * 4 + jc4
            if jc4 == 0 and t2 == 0:
                slot, B_slot = w_next()
                sl = slot[:].rearrange("p (k n) -> p k n", n=512)
            if i + 2 < len(steps):
                xload(i + 2)
            bi = rot["mm"] % 4
            rot["mm"] += 1
            K.mm([lambda e, kc=kc: e.matmul(ps[bi][:], sl[:, kc, jc4 * 128:(jc4 + 1) * 128],
                                            mixh[:, kc, t2 * TT:(t2 + 1) * TT], start=(kc == 0), stop=(kc == KC - 1))
                  for kc in range(KC)], [B_slot, B_mixh2[t2]], [B_ps[bi]])
            xi = i % 2
            K.op(dve, lambda e: e.tensor_tensor(out=x1t[xi], in0=ps[bi][:], in1=xr[i % 4], op=ALU.add),
                 [B_ps[bi], B_xr[i % 4]], [B_x1t[xi]])
            K.dma(sp, x1d[jc * 128:(jc + 1) * 128, hoff + t2 * TT:hoff + (t2 + 1) * TT], x1t[xi], B_x1t[xi], False)
            K.op(act, lambda e: e.activation(out=h2p[:, jc, t2 * TT:(t2 + 1) * TT], in_=x1t[xi], func=AF.Copy,
                                             scale=cf[:, CF_G2 + jc:CF_G2 + jc + 1]), [B_x1t[xi], B_cf], [B_h2p])
            K.op(act, lambda e: e.activation(out=sq2[xi], in_=x1t[xi], func=AF.Square), [B_x1t[xi]], [B_sq2[xi]])
            if pend_ssq is not None:
                pend_ssq()
            pend_ssq = (lambda t2=t2, xi=xi, jc=jc: K.mm(
                [lambda e: e.matmul(ps[6 + t2][:], ones, sq2[xi], start=(jc == 0), stop=(jc == KC - 1))],
                [B_sq2[xi], B_cb], [B_ps[6 + t2]]))
            if jc4 == 3 and t2 == 1:
                w_issue()
        pend_ssq()
        for t2 in range(2):
            K.op(act, lambda e: e.activation(out=rs2[:, t2 * TT:(t2 + 1) * TT], in_=ps[6 + t2][:], func=AF.Ln,
                                             scale=1.0 / D, bias=EPS), [B_ps[6 + t2]], [B_rs2])
        K.op(act, lambda e: e.activation(out=rs2, in_=rs2, func=AF.Exp, scale=-0.5), [B_rs2], [B_rs2])
        if stage == "C" and hf == 1:
            for b in K.dbufs:
                if b.dcnt > 0:
                    sp.wait((b.dsem, b.dcnt))
            return nc
        K.barrier()
        yi = 0
        for ub in range(22):
            slot, B_slot = w_next()
            sl = slot[:].rearrange("p (k n) -> p k n", n=512)
            for pi in range(2):
                cp = 2 * ub + pi
                si = yi % 2
                yi += 1
                for gv in range(2):
                    j = cp + NFC * gv
                    y, B_yy = ybuf[si][gv], B_y[si][gv]
                    if hf == 0:
                        K.op(dve, lambda e: e.memset(y[:, 0:2], 0.0), [], [B_yy])
                    else:
                        K.op(act, lambda e: e.activation(out=y[:, 0:2], in_=halo[:, j, :], func=AF.Copy),
                             [B_halo], [B_yy])
                    for t2 in range(2):
                        bi = rot["mm"] % 4
                        rot["mm"] += 1
                        c0 = gv * 256 + pi * 128
                        K.mm([lambda e, kc=kc: e.matmul(ps[bi][:], sl[:, kc, c0:c0 + 128], h2p[:, kc, t2 * TT:(t2 + 1) * TT],
                                                        start=(kc == 0), stop=(kc == KC - 1)) for kc in range(KC)],
                             [B_slot, B_h2p], [B_ps[bi]])
                        K.op(dve, lambda e: e.tensor_tensor(out=y[:, 2 + t2 * TT:2 + (t2 + 1) * TT], in0=ps[bi][:],
                                                            in1=rs2[:, t2 * TT:(t2 + 1) * TT], op=ALU.mult),
                             [B_ps[bi], B_rs2], [B_yy])
                    if hf == 0:
                        K.op(act, lambda e: e.activation(out=halo[:, j, :], in_=y[:, HS:HS + 2], func=AF.Copy),
                             [B_yy], [B_halo])
                    a, B_a = (ag, B_ag) if gv == 0 else (av, B_av)
                    wc = lambda k: cf[:, CF_FCW + j * 3 + k:CF_FCW + j * 3 + k + 1]
                    K.op(act, lambda e: e.activation(out=a, in_=y[:, 2:2 + HS], func=AF.Identity, scale=wc(2),
                                                     bias=cf[:, CF_FCB + j:CF_FCB + j + 1]), [B_yy, B_cf], [B_a])
                    for k in (1, 0):
                        K.op(dve, lambda e, k=k: e.scalar_tensor_tensor(out=a, in0=y[:, k:k + HS], scalar=wc(k), in1=a,
                                                                        op0=ALU.mult, op1=ALU.add),
                             [B_yy, B_cf, B_a], [B_a])
                K.op(act, lambda e: e.activation(out=ag, in_=ag, func=AF.Silu), [B_ag], [B_ag])
                K.op(dve, lambda e: e.tensor_tensor(out=actT[:, cp, :], in0=ag, in1=av, op=ALU.mult),
                     [B_ag, B_av], [B_actT])
            w_issue()
        for jb2 in range(8):
            base = 0 if jb2 % 2 == 0 else 4
            for q4 in range(4):
                jc2, t2 = q4 // 2, q4 % 2
                jc = jb2 * 2 + jc2
                K.dma(sp, xr2[q4 % 2], x1d[jc * 128:(jc + 1) * 128, hoff + t2 * TT:hoff + (t2 + 1) * TT], B_xr2[q4 % 2], True) \
                    if q4 < 2 else None
            for kh in range(2):
                slot, B_slot = w_next()
                slv = slot[:, 0:22 * 256].rearrange("p (k n) -> p k n", n=256)
                for q4 in range(4):
                    jc2, t2 = q4 // 2, q4 % 2
                    bi = base + q4
                    K.mm([lambda e, k=k: e.matmul(ps[bi][:], slv[:, k, jc2 * 128:(jc2 + 1) * 128],
                                                  actT[:, kh * 22 + k, t2 * TT:(t2 + 1) * TT],
                                                  start=(kh == 0 and k == 0), stop=(kh == 1 and k == 21))
                          for k in range(22)], [B_slot, B_actT], [B_ps[bi]])
                w_issue()
            for q4 in range(4):
                jc2, t2 = q4 // 2, q4 % 2
                jc = jb2 * 2 + jc2
                bi = base + q4
                K.op(dve, lambda e: e.tensor_tensor(out=otb[q4 % 2], in0=ps[bi][:], in1=xr2[q4 % 2], op=ALU.add),
                     [B_ps[bi], B_xr2[q4 % 2]], [B_ot[q4 % 2]])
                K.dma(sp, outT[jc * 128:(jc + 1) * 128, hoff + t2 * TT:hoff + (t2 + 1) * TT], otb[q4 % 2], B_ot[q4 % 2], False)
                if q4 + 2 < 4:
                    jc2n, t2n = (q4 + 2) // 2, (q4 + 2) % 2
                    jcn = jb2 * 2 + jc2n
                    K.dma(sp, xr2[q4 % 2], x1d[jcn * 128:(jcn + 1) * 128, hoff + t2n * TT:hoff + (t2n + 1) * TT],
                          B_xr2[q4 % 2], True)

    for b in K.dbufs:
        if b.dcnt > 0:
            sp.wait((b.dsem, b.dcnt))
    return nc

def _consts():
    cbn = np.zeros((P, NCB), np.float32)
    i = np.arange(P)
    cbn[i, CB_ID + i] = 1.0
    cbn[:, CB_ONE:CB_ONE + 128] = 1.0
    for m in range(64):
        cbn[m + 64, CB_PSG + m] = -1.0
        cbn[m, CB_PSG + m + 64] = 1.0
    ik = np.arange(P)[:, None]
    iq = np.arange(P)[None, :]
    own = (ik <= iq).astype(np.float32)
    prev = (ik >= iq).astype(np.float32)
    cbn[:, CB_MOWN:CB_MOWN + 512] = np.tile(own, (1, 4))
    cbn[:, CB_MPREV:CB_MPREV + 512] = np.tile(prev, (1, 4))
    for T in range(4):
        m3 = (np.arange(P)[:, None] <= (32 * T + np.arange(32))[None, :]).astype(np.float32)
        cbn[:, CB_M3 + T * 512:CB_M3 + (T + 1) * 512] = np.repeat(m3, 16, axis=1)
    cbn[:, CB_MOWN4:CB_MOWN4 + 512] = np.repeat(own, 4, axis=1)
    cbn[:, CB_MPREV4:CB_MPREV4 + 512] = np.repeat(prev, 4, axis=1)
    half = 64
    inv_freq = (10000.0 ** (-np.arange(half, dtype=np.float32) / half)).astype(np.float32)
    ang = np.arange(S, dtype=np.float32)[None, :] * inv_freq[:, None]
    cos = np.cos(ang).astype(np.float32)
    sin = np.sin(ang).astype(np.float32)
    cs = np.concatenate([np.concatenate([cos, cos], 0), np.concatenate([sin, sin], 0)], 1)
    return cbn, np.ascontiguousarray(cs)


def _params(inp):
    cfn = np.zeros((P, NCF), np.float32)
    i = np.arange(P)
    cfn[:, CF_TRIU:CF_TRIU + 128] = (i[:, None] <= i[None, :]).astype(np.float32)
    cfn[:, CF_G1:CF_G1 + 16] = inp["norm_mix_g"][0].reshape(16, P).T
    cfn[:, CF_G2:CF_G2 + 16] = inp["norm_ffn_g"][0].reshape(16, P).T
    cfn[:, CF_QG] = inp["q_norm_g"][0]
    cfn[:, CF_KG] = inp["k_norm_g"][0]
    cw = inp["mlstm_conv_w"][0]
    cfn[:, CF_MCW:CF_MCW + 32] = cw.reshape(4, 8, P).transpose(2, 1, 0).reshape(P, 32)
    cfn[:, CF_MCB:CF_MCB + 8] = inp["mlstm_conv_b"][0].reshape(8, P).T
    cfn[:, CF_BG:CF_BG + 4] = inp["b_igate"][0][None, :]
    cfn[:, CF_BG + 4:CF_BG + 8] = inp["b_fgate"][0][None, :]
    cfn[:, CF_MNG:CF_MNG + 4] = inp["mlstm_norm_g"][0].reshape(4, P).T
    fw = inp["ffn_conv_w"][0]
    cfn[:, CF_FCW:CF_FCW + 264] = fw.reshape(3, 88, P).transpose(2, 1, 0).reshape(P, 264)
    cfn[:, CF_FCB:CF_FCB + 88] = inp["ffn_conv_b"][0].reshape(88, P).T
    return cfn


_NC_CACHE = {}


def run(inputs, stage="FULL", ncores=8):
    inp = {k: np.asarray(v) for k, v in inputs.items()}
    if stage not in _NC_CACHE:
        _NC_CACHE[stage] = build(stage)
    nc = _NC_CACHE[stage]
    cbn, cs = _consts()
    cfn = _params(inp)
    w_in = np.ascontiguousarray(inp["w_in"][0])
    w_out = np.ascontiguousarray(inp["w_out"][0])
    w_up = np.ascontiguousarray(inp["w_up"][0])
    w_down = np.ascontiguousarray(inp["w_down"][0])
    in_maps = []
    for b in range(ncores):
        in_maps.append({"xT": np.ascontiguousarray(inp["x"][b].T), "w_in": w_in, "w_out": w_out, "w_up": w_up,
                        "w_down": w_down, "cb": cbn, "cf": cfn, "cs": cs})
    res = run_bass_kernel_spmd(nc, in_maps, core_ids=list(range(ncores)))
    return res


def kernel(**inputs):
    res = run(inputs, "FULL", 8)
    out = np.stack([np.ascontiguousarray(r["outT"].T) for r in res.results], axis=0)
    return out.astype(np.float32)
```
